# Optimizing a Trainium2 kernel written in Bass

```python
import math
import jax, jax.numpy as jnp
from jax import lax
import numpy as np

D_MODEL = 1024
BATCH = 8
SEQ = 4096
DEPTH = 2

GRID_W = 64
CTX_LEN = 256
HEAD_DIM = 64
NA_HEADS = 4
NA_WIN_R = 8
NA_WIN_C = 16
GQA_Q_HEADS = 4
GQA_KV_HEADS = 2
DN_HEADS = 4
DN_CONV = 5
DN_CHUNK = 64
MLA_HEADS = 4
MLA_Q_RANK = 256
MLA_KV_RANK = 128
MLA_NOPE = 64
MLA_ROPE = 32
MLA_V = 64
MLA_QK = MLA_NOPE + MLA_ROPE
N_EXPERTS = 32
TOP_K = 4
D_EXPERT = 1024
SWIGLU_LIMIT = 7.0
SWIGLU_ALPHA = 1.702
ROPE_THETA = 10000.0
EPS = 1e-6
NEG_INF = -1e30
Q_BLOCK = 128
N_ADA = 6

NA_W = NA_HEADS * HEAD_DIM
GQA_QW = GQA_Q_HEADS * HEAD_DIM
GQA_KVW = GQA_KV_HEADS * HEAD_DIM
DN_W = DN_HEADS * HEAD_DIM
MLA_W = MLA_HEADS * MLA_V
MIX_W = NA_W + GQA_QW + DN_W + MLA_W
IN_SIZES = (NA_W, NA_W, NA_W, GQA_QW, GQA_KVW, GQA_KVW, DN_W, DN_W, DN_W, DN_W, 2 * DN_HEADS, 2 * DN_HEADS, MLA_Q_RANK, MLA_KV_RANK, MLA_ROPE)
IN_OFFSETS = tuple(int(o) for o in np.cumsum(IN_SIZES)[:-1])
IN_W = int(sum(IN_SIZES))

kernel_name = 'hybrid_natten_gqa_gdn_mla_moe_dit'


def rms_norm(x, g):
    xf = x.astype(jnp.float32)
    y = xf * lax.rsqrt(jnp.mean(jnp.square(xf), axis=-1, keepdims=True) + EPS)
    return (y * g.astype(jnp.float32)).astype(x.dtype)


def l2_norm(x):
    xf = x.astype(jnp.float32)
    return (xf * lax.rsqrt(jnp.sum(jnp.square(xf), axis=-1, keepdims=True) + EPS)).astype(x.dtype)


def modulate(h, shift, scale):
    return h * (1 + scale) + shift


def split_heads(t, n):
    return t.reshape(t.shape[0], t.shape[1], n, t.shape[2] // n)


def axial_rope_tables(n_tok, rot_dim):
    t = jnp.arange(n_tok, dtype=jnp.int32)
    row = (t // GRID_W).astype(jnp.float32)
    col = (t % GRID_W).astype(jnp.float32)
    n_freq = rot_dim // 4
    freqs = ROPE_THETA ** (-jnp.arange(n_freq, dtype=jnp.float32) / n_freq)
    ang = jnp.concatenate([row[:, None] * freqs, col[:, None] * freqs], axis=-1)
    return jnp.cos(ang), jnp.sin(ang)


def apply_rope(x, cos, sin):
    xf = x.astype(jnp.float32).reshape(*x.shape[:-1], -1, 2)
    x1, x2 = xf[..., 0], xf[..., 1]
    cs, sn = cos[:, None, :], sin[:, None, :]
    out = jnp.stack([x1 * cs - x2 * sn, x1 * sn + x2 * cs], axis=-1)
    return out.reshape(x.shape).astype(x.dtype)


def rope_tail(t, cos, sin):
    return jnp.concatenate([t[..., :MLA_NOPE], apply_rope(t[..., MLA_NOPE:], cos, sin)], axis=-1)


def blocked_attention(q, k, v):
    B, N, G, R, dk = q.shape
    nb = N // Q_BLOCK
    scale = dk ** -0.5
    qb = jnp.moveaxis(q.reshape(B, nb, Q_BLOCK, G, R, dk), 1, 0)

    def one_block(qi):
        s = jnp.einsum('bqgrd,bkgd->bgrqk', qi, k).astype(jnp.float32) * scale
        p = jax.nn.softmax(s, axis=-1).astype(v.dtype)
        return jnp.einsum('bgrqk,bkge->bqgre', p, v)

    o = lax.map(one_block, qb)
    return jnp.moveaxis(o, 0, 1).reshape(B, N, G, R, v.shape[-1])


def neighbourhood_attention(q, k, v, kc, vc, rel_bias):
    B, N, H, d = q.shape
    rows = N // GRID_W
    kr = min(NA_WIN_R, rows)
    scale = d ** -0.5
    qg = q.reshape(B, rows, GRID_W, H, d)
    kg = k.reshape(B, rows, GRID_W, H, d)
    vg = v.reshape(B, rows, GRID_W, H, d)
    r = jnp.arange(rows)
    r0 = jnp.clip(r - kr // 2, 0, rows - kr)
    ridx = r0[:, None] + jnp.arange(kr)[None, :]
    kw = kg[:, ridx]
    vw = vg[:, ridx]
    cpos = jnp.arange(GRID_W)
    c0 = jnp.clip(cpos - NA_WIN_C // 2, 0, GRID_W - NA_WIN_C)
    col_ok = (cpos[None, :] >= c0[:, None]) & (cpos[None, :] < c0[:, None] + NA_WIN_C)
    dr = ridx - r[:, None] + (NA_WIN_R - 1)
    dc = jnp.clip(cpos[None, :] - cpos[:, None], -(NA_WIN_C - 1), NA_WIN_C - 1) + (NA_WIN_C - 1)
    bias = rel_bias[:, dr[:, None, :, None], dc[None, :, None, :]]
    s_loc = jnp.einsum('brchd,brkwhd->bhrckw', qg, kw).astype(jnp.float32) * scale + bias.astype(jnp.float32)
    s_loc = jnp.where(col_ok[:, None, :], s_loc, NEG_INF)
    s_ctx = jnp.einsum('brchd,blhd->bhrcl', qg, kc).astype(jnp.float32) * scale
    s = jnp.concatenate([s_loc.reshape(B, H, rows, GRID_W, kr * GRID_W), s_ctx], axis=-1)
    p = jax.nn.softmax(s, axis=-1).astype(v.dtype)
    p_loc = p[..., :kr * GRID_W].reshape(B, H, rows, GRID_W, kr, GRID_W)
    p_ctx = p[..., kr * GRID_W:]
    o = jnp.einsum('bhrckw,brkwhd->brchd', p_loc, vw) + jnp.einsum('bhrcl,blhd->brchd', p_ctx, vc)
    return o.reshape(B, N, H, d)


def short_conv(x, w):
    C = x.shape[-1]
    return lax.conv_general_dilated(x, w[:, None, :].astype(x.dtype), window_strides=(1,),
                                    padding=[(DN_CONV // 2, DN_CONV // 2)],
                                    dimension_numbers=('NWC', 'WIO', 'NWC'), feature_group_count=C)


def dn_inputs(q, k, v, beta_raw, a_raw, conv_w, a_log, dt_bias):
    B, N, _ = q.shape
    qkv = jax.nn.silu(short_conv(jnp.concatenate([q, k, v], axis=-1), conv_w))
    q, k, v = [split_heads(t, DN_HEADS) for t in jnp.split(qkv, 3, axis=-1)]
    beta = jax.nn.sigmoid(beta_raw.astype(jnp.float32)).reshape(B, N, 2, DN_HEADS)
    g = -jnp.exp(a_log.astype(jnp.float32)) * jax.nn.softplus(
        a_raw.astype(jnp.float32).reshape(B, N, 2, DN_HEADS) + dt_bias.astype(jnp.float32))
    return l2_norm(q), l2_norm(k), v, g, beta


def gated_delta_chunked(q, k, v, g, beta, s0):
    B, N, H, dk = q.shape
    dv = v.shape[-1]
    nc = N // DN_CHUNK

    def chunks(t):
        t = t.astype(jnp.float32).reshape(B, nc, DN_CHUNK, H, *t.shape[3:])
        return jnp.moveaxis(jnp.moveaxis(t, 1, 0), 3, 2)

    qch = chunks(q) * dk ** -0.5
    kch = chunks(k)
    vch = chunks(v)
    gc = jnp.cumsum(chunks(g), axis=-1)
    bch = chunks(beta)
    i = jnp.arange(DN_CHUNK)
    lower = i[:, None] >= i[None, :]
    strict = i[:, None] > i[None, :]
    decay = jnp.exp(jnp.where(lower, gc[..., :, None] - gc[..., None, :], NEG_INF))
    kb = kch * bch[..., None]
    a = jnp.where(strict, jnp.einsum('nbhid,nbhjd->nbhij', kb, kch) * decay, 0.0)
    eye = jnp.eye(DN_CHUNK, dtype=jnp.float32)
    t_inv = lax.linalg.triangular_solve(eye + a, jnp.broadcast_to(eye, a.shape), left_side=True,
                                        lower=True, unit_diagonal=True)
    u = jnp.einsum('nbhij,nbhje->nbhie', t_inv, vch * bch[..., None])
    w = jnp.einsum('nbhij,nbhjd->nbhid', t_inv, kb * jnp.exp(gc)[..., None])
    attn = jnp.where(lower, jnp.einsum('nbhid,nbhjd->nbhij', qch, kch) * decay, 0.0)

    def step(S, inp):
        qi, ki, ui, wi, gi, ai = inp
        v_new = ui - jnp.einsum('bhcd,bhde->bhce', wi, S)
        o = jnp.einsum('bhcd,bhde->bhce', qi * jnp.exp(gi)[..., None], S) + jnp.einsum('bhij,bhje->bhie', ai, v_new)
        g_last = gi[..., -1]
        k_dec = ki * jnp.exp(g_last[..., None] - gi)[..., None]
        S = S * jnp.exp(g_last)[..., None, None] + jnp.einsum('bhcd,bhce->bhde', k_dec, v_new)
        return S, o

    s_final, o = lax.scan(step, s0.astype(jnp.float32), (qch, kch, u, w, gc, attn))
    o = jnp.moveaxis(jnp.moveaxis(o, 2, 3), 0, 1).reshape(B, N, H, dv)
    return o.astype(v.dtype), s_final


def bidir_scan(q, k, v, g, beta, s0_f, s0_b):
    o_f, s_f = gated_delta_chunked(q, k, v, g[:, :, 0], beta[:, :, 0], s0_f)
    o_b, s_b = gated_delta_chunked(jnp.flip(q, 1), jnp.flip(k, 1), jnp.flip(v, 1),
                                   jnp.flip(g[:, :, 1], 1), jnp.flip(beta[:, :, 1], 1), s0_b)
    return o_f + jnp.flip(o_b, 1), s_f, s_b


def mla_queries(cq, cq_g, w_uq, qn_g):
    B, N, _ = cq.shape
    q = (rms_norm(cq, cq_g) @ w_uq).reshape(B, N, MLA_HEADS, MLA_QK)
    return rms_norm(q, qn_g)


def mla_keys_values(ckv, kpe, ckv_g, w_ukv, kn_g):
    B, N, _ = ckv.shape
    kv = (rms_norm(ckv, ckv_g) @ w_ukv).reshape(B, N, MLA_HEADS, MLA_NOPE + MLA_V)
    k_pe = jnp.broadcast_to(kpe[:, :, None, :], (B, N, MLA_HEADS, MLA_ROPE))
    k = jnp.concatenate([kv[..., :MLA_NOPE], k_pe], axis=-1)
    return rms_norm(k, kn_g), kv[..., MLA_NOPE:]


def moe_ffn(t, router_w, router_b, w1, b1, w2, b2):
    logits = (t @ router_w + router_b).astype(jnp.float32)
    top_val, top_idx = lax.top_k(logits, TOP_K)
    gates = jax.nn.softmax(top_val, axis=-1)
    combine = jnp.sum(jax.nn.one_hot(top_idx, N_EXPERTS, dtype=jnp.float32) * gates[..., None], axis=1).astype(t.dtype)
    out = jnp.zeros_like(t)
    for e in range(N_EXPERTS):
        hh = t @ w1[e] + b1[e]
        glu = jnp.minimum(hh[:, :D_EXPERT], SWIGLU_LIMIT)
        lin = jnp.clip(hh[:, D_EXPERT:], -SWIGLU_LIMIT, SWIGLU_LIMIT)
        act = glu * jax.nn.sigmoid(SWIGLU_ALPHA * glu) * (lin + 1)
        out = out + combine[:, e:e + 1] * (act @ w2[e] + b2[e])
    return out


def setup_inputs(seed: int = 0) -> dict:
    key = jax.random.key(seed)
    ks = jax.random.split(key, 40)
    f32 = jnp.float32
    D = D_MODEL

    def nrm(k, shape, s):
        return jax.random.normal(k, shape, f32) * s

    dt = jnp.exp(jax.random.uniform(ks[17], (DEPTH, 2, DN_HEADS), f32, math.log(1e-3), math.log(1e-1)))
    return {
        'x': nrm(ks[0], (BATCH, SEQ, D), 1.0),
        'c': nrm(ks[1], (BATCH, D), 1.0),
        'ctx': nrm(ks[2], (BATCH, CTX_LEN, D), 1.0),
        'c_ctx': nrm(ks[3], (D,), 1.0),
        'ada_w': nrm(ks[4], (DEPTH, D, N_ADA * D), 0.5 * D ** -0.5),
        'ada_b': nrm(ks[5], (DEPTH, N_ADA * D), 0.02),
        'norm1_g': 1.0 + nrm(ks[6], (DEPTH, D), 0.02),
        'norm2_g': 1.0 + nrm(ks[7], (DEPTH, D), 0.02),
        'w_in': nrm(ks[8], (DEPTH, D, IN_W), D ** -0.5),
        'w_out': nrm(ks[9], (DEPTH, MIX_W, D), MIX_W ** -0.5),
        'na_qn_g': 1.0 + nrm(ks[10], (DEPTH, HEAD_DIM), 0.02),
        'na_kn_g': 1.0 + nrm(ks[11], (DEPTH, HEAD_DIM), 0.02),
        'na_rel_bias': nrm(ks[12], (DEPTH, NA_HEADS, 2 * NA_WIN_R - 1, 2 * NA_WIN_C - 1), 0.1),
        'gqa_qn_g': 1.0 + nrm(ks[13], (DEPTH, HEAD_DIM), 0.02),
        'gqa_kn_g': 1.0 + nrm(ks[14], (DEPTH, HEAD_DIM), 0.02),
        'dn_conv_w': nrm(ks[15], (DEPTH, DN_CONV, 3 * DN_W), DN_CONV ** -0.5),
        'dn_a_log': jnp.log(jax.random.uniform(ks[16], (DEPTH, 2, DN_HEADS), f32, 1.0, 16.0)),
        'dn_dt_bias': dt + jnp.log(-jnp.expm1(-dt)),
        'dn_out_g': 1.0 + nrm(ks[18], (DEPTH, HEAD_DIM), 0.02),
        'mla_cq_g': 1.0 + nrm(ks[19], (DEPTH, MLA_Q_RANK), 0.02),
        'mla_ckv_g': 1.0 + nrm(ks[20], (DEPTH, MLA_KV_RANK), 0.02),
        'mla_w_uq': nrm(ks[21], (DEPTH, MLA_Q_RANK, MLA_HEADS * MLA_QK), MLA_Q_RANK ** -0.5),
        'mla_w_ukv': nrm(ks[22], (DEPTH, MLA_KV_RANK, MLA_HEADS * (MLA_NOPE + MLA_V)), MLA_KV_RANK ** -0.5),
        'mla_qn_g': 1.0 + nrm(ks[23], (DEPTH, MLA_QK), 0.02),
        'mla_kn_g': 1.0 + nrm(ks[24], (DEPTH, MLA_QK), 0.02),
        'router_w': nrm(ks[25], (DEPTH, D, N_EXPERTS), D ** -0.5),
        'router_b': nrm(ks[26], (DEPTH, N_EXPERTS), 0.01),
        'exp_w1': nrm(ks[27], (DEPTH, N_EXPERTS, D, 2 * D_EXPERT), D ** -0.5),
        'exp_b1': nrm(ks[28], (DEPTH, N_EXPERTS, 2 * D_EXPERT), 0.01),
        'exp_w2': nrm(ks[29], (DEPTH, N_EXPERTS, D_EXPERT, D), D_EXPERT ** -0.5),
        'exp_b2': nrm(ks[30], (DEPTH, N_EXPERTS, D), 0.01),
    }


def reference(x, c, ctx, c_ctx, ada_w, ada_b, norm1_g, norm2_g, w_in, w_out, na_qn_g, na_kn_g, na_rel_bias,
              gqa_qn_g, gqa_kn_g, dn_conv_w, dn_a_log, dn_dt_bias, dn_out_g, mla_cq_g, mla_ckv_g, mla_w_uq,
              mla_w_ukv, mla_qn_g, mla_kn_g, router_w, router_b, exp_w1, exp_b1, exp_w2, exp_b2):
    B, N, D = x.shape
    L = ctx.shape[1]
    grp = GQA_Q_HEADS // GQA_KV_HEADS
    cos_g, sin_g = axial_rope_tables(N, HEAD_DIM)
    cos_m, sin_m = axial_rope_tables(N, MLA_ROPE)
    sc = jax.nn.silu(c)
    scc = jax.nn.silu(c_ctx)
    s0 = jnp.zeros((B, DN_HEADS, HEAD_DIM, HEAD_DIM), jnp.float32)
    for l in range(DEPTH):
        with_ctx = l < DEPTH - 1
        mod = jnp.split(sc @ ada_w[l] + ada_b[l], N_ADA, axis=-1)
        mod_c = jnp.split(scc @ ada_w[l] + ada_b[l], N_ADA, axis=-1)
        h = modulate(rms_norm(x, norm1_g[l]), mod[0][:, None], mod[1][:, None])
        hc = modulate(rms_norm(ctx, norm1_g[l]), mod_c[0], mod_c[1])
        p = jnp.split(h @ w_in[l], IN_OFFSETS, axis=-1)
        pc = jnp.split(hc @ w_in[l], IN_OFFSETS, axis=-1)

        qa = rms_norm(split_heads(p[0], NA_HEADS), na_qn_g[l])
        ka = rms_norm(split_heads(p[1], NA_HEADS), na_kn_g[l])
        va = split_heads(p[2], NA_HEADS)
        kac = rms_norm(split_heads(pc[1], NA_HEADS), na_kn_g[l])
        vac = split_heads(pc[2], NA_HEADS)
        ya = neighbourhood_attention(qa, ka, va, kac, vac, na_rel_bias[l]).reshape(B, N, NA_W)

        qb = apply_rope(rms_norm(split_heads(p[3], GQA_Q_HEADS), gqa_qn_g[l]), cos_g, sin_g)
        kb = apply_rope(rms_norm(split_heads(p[4], GQA_KV_HEADS), gqa_kn_g[l]), cos_g, sin_g)
        vb = split_heads(p[5], GQA_KV_HEADS)
        kbc = rms_norm(split_heads(pc[4], GQA_KV_HEADS), gqa_kn_g[l])
        vbc = split_heads(pc[5], GQA_KV_HEADS)
        yb = blocked_attention(qb.reshape(B, N, GQA_KV_HEADS, grp, HEAD_DIM),
                               jnp.concatenate([kb, kbc], axis=1), jnp.concatenate([vb, vbc], axis=1)).reshape(B, N, GQA_QW)

        qdc, kdc, vdc, gdc, bdc = dn_inputs(pc[6], pc[7], pc[8], pc[10], pc[11], dn_conv_w[l], dn_a_log[l], dn_dt_bias[l])
        odc, s_f, s_b = bidir_scan(qdc, kdc, vdc, gdc, bdc, s0, s0)
        qd, kd, vd, gd, bd = dn_inputs(p[6], p[7], p[8], p[10], p[11], dn_conv_w[l], dn_a_log[l], dn_dt_bias[l])
        od, _, _ = bidir_scan(qd, kd, vd, gd, bd, s_f, s_b)
        yc = (rms_norm(od, dn_out_g[l]) * jax.nn.silu(split_heads(p[9], DN_HEADS))).reshape(B, N, DN_W)

        qm = rope_tail(mla_queries(p[12], mla_cq_g[l], mla_w_uq[l], mla_qn_g[l]), cos_m, sin_m)
        km, vm = mla_keys_values(p[13], p[14], mla_ckv_g[l], mla_w_ukv[l], mla_kn_g[l])
        km = rope_tail(km, cos_m, sin_m)
        kmc, vmc = mla_keys_values(pc[13], pc[14], mla_ckv_g[l], mla_w_ukv[l], mla_kn_g[l])
        yd = blocked_attention(qm[:, :, :, None], jnp.concatenate([km, kmc], axis=1),
                               jnp.concatenate([vm, vmc], axis=1)).reshape(B, N, MLA_W)

        x = x + mod[2][:, None] * (jnp.concatenate([ya, yb, yc, yd], axis=-1) @ w_out[l])
        h2 = modulate(rms_norm(x, norm2_g[l]), mod[3][:, None], mod[4][:, None])

        if with_ctx:
            qac = rms_norm(split_heads(pc[0], NA_HEADS), na_qn_g[l])
            yac = blocked_attention(qac[:, :, :, None], kac, vac).reshape(B, L, NA_W)
            qbc = rms_norm(split_heads(pc[3], GQA_Q_HEADS), gqa_qn_g[l])
            ybc = blocked_attention(qbc.reshape(B, L, GQA_KV_HEADS, grp, HEAD_DIM), kbc, vbc).reshape(B, L, GQA_QW)
            ycc = (rms_norm(odc, dn_out_g[l]) * jax.nn.silu(split_heads(pc[9], DN_HEADS))).reshape(B, L, DN_W)
            qmc = mla_queries(pc[12], mla_cq_g[l], mla_w_uq[l], mla_qn_g[l])
            ydc = blocked_attention(qmc[:, :, :, None], kmc, vmc).reshape(B, L, MLA_W)
            ctx = ctx + mod_c[2] * (jnp.concatenate([yac, ybc, ycc, ydc], axis=-1) @ w_out[l])
            h2c = modulate(rms_norm(ctx, norm2_g[l]), mod_c[3], mod_c[4])
            tok = jnp.concatenate([h2.reshape(B * N, D), h2c.reshape(B * L, D)], axis=0)
            f = moe_ffn(tok, router_w[l], router_b[l], exp_w1[l], exp_b1[l], exp_w2[l], exp_b2[l])
            x = x + mod[5][:, None] * f[:B * N].reshape(B, N, D)
            ctx = ctx + mod_c[5] * f[B * N:].reshape(B, L, D)
        else:
            f = moe_ffn(h2.reshape(B * N, D), router_w[l], router_b[l], exp_w1[l], exp_b1[l], exp_w2[l], exp_b2[l])
            x = x + mod[5][:, None] * f.reshape(B, N, D)
    return x
```

```python
import contextlib
import numpy as np
import concourse.bass as bass
import concourse.mybir as mybir
from concourse.bass_utils import run_bass_kernel_spmd

F32 = mybir.dt.float32
BF16 = mybir.dt.bfloat16
ALU = mybir.AluOpType
AF = mybir.ActivationFunctionType
AX = mybir.AxisListType

D = 1024
NLAT = 4096
NCTX = 256
T = NLAT + NCTX
NT = T // 128
NTL = NLAT // 128
DEPTH = 2
INW = 2736
NE = 32
EPS = 1e-6
MASKNEG = -30000.0


def _key(x):
    return x if isinstance(x, str) else x.name


class Sched:
    NDMA = 12

    def __init__(self, nc, stack):
        self.nc = nc
        self.E = {"pe": nc.tensor, "dve": nc.vector, "act": nc.scalar, "pool": nc.gpsimd, "sp": nc.sync}
        self.sem, self.cnt = {}, {}
        for e in ("pe", "dve", "act", "pool"):
            self.sem[e] = stack.enter_context(nc.semaphore("s_" + e))
            self.cnt[e] = 0
        self.dsem, self.dval, self.dnext = {}, {}, {}
        for q in ("sp", "act", "pool"):
            self.dsem[q] = [stack.enter_context(nc.semaphore(f"d_{q}{i}")) for i in range(self.NDMA)]
            self.dval[q] = [0] * self.NDMA
            self.dnext[q] = 0
        self.waited, self.semobj, self.W, self.R = {}, {}, {}, {}
        self.psum_keys = set()
        self.ninst = 0
        self.out_tokens = []

    def _tok(self, sem, val):
        self.semobj[sem.name] = sem
        return (sem.name, val)

    def _wait(self, eng, toks):
        need = {}
        for t in toks:
            if t is None:
                continue
            n, v = t
            if v > need.get(n, 0):
                need[n] = v
        for n, v in need.items():
            k = (eng, n)
            if self.waited.get(k, 0) >= v:
                continue
            if eng == "pe" and n == "s_pe":
                continue
            self.E[eng].wait_ge(self.semobj[n], v)
            self.waited[k] = v
            self.ninst += 1

    def _deps(self, reads, writes):
        toks = []
        for k in reads:
            toks.append(self.W.get(k))
            if k in self.psum_keys:
                toks.extend(self.R.get(k, ()))
        for k in writes:
            toks.append(self.W.get(k))
            toks.extend(self.R.get(k, ()))
        return toks

    def _commit(self, tok, reads, writes):
        for k in reads:
            lst = self.R.setdefault(k, [])
            lst.append(tok)
            if len(lst) > 16:
                best = {}
                for n, v in lst:
                    if v > best.get(n, 0):
                        best[n] = v
                self.R[k] = list(best.items())
        for k in writes:
            self.W[k] = tok
            self.R[k] = []

    def op(self, eng, fn, reads=(), writes=()):
        reads = [_key(r) for r in reads]
        writes = [_key(w) for w in writes]
        self._wait(eng, self._deps(reads, writes))
        ins = fn()
        self.cnt[eng] += 1
        ins.then_inc(self.sem[eng], 1)
        tok = self._tok(self.sem[eng], self.cnt[eng])
        self._commit(tok, reads, writes)
        self.ninst += 1
        return tok

    def dma(self, q, out, in_, reads=(), writes=(), is_output=False):
        reads = [_key(r) for r in reads]
        writes = [_key(w) for w in writes]
        i = self.dnext[q]
        self.dnext[q] = (i + 1) % self.NDMA
        sem = self.dsem[q][i]
        prev = self._tok(sem, self.dval[q][i]) if self.dval[q][i] else None
        self._wait(q, self._deps(reads, writes) + [prev])
        ins = self.E[q].dma_start(out=out, in_=in_)
        self.dval[q][i] += 16
        ins.then_inc(sem, 16)
        tok = self._tok(sem, self.dval[q][i])
        self._commit(tok, reads, writes)
        self.ninst += 1
        if is_output:
            self.out_tokens.append(tok)
        return tok

    def _all_tokens(self):
        toks = []
        for e in ("pe", "dve", "act", "pool"):
            if self.cnt[e]:
                toks.append(self._tok(self.sem[e], self.cnt[e]))
        for q in self.dsem:
            for i, s in enumerate(self.dsem[q]):
                if self.dval[q][i]:
                    toks.append(self._tok(s, self.dval[q][i]))
        return toks

    def barrier(self):
        toks = self._all_tokens()
        for e in ("pe", "dve", "act", "pool", "sp"):
            self._wait(e, toks)
        self.W, self.R = {}, {}

    def finish(self):
        self._wait("sp", self._all_tokens() + self.out_tokens)


def _rope_tables(rot_dim):
    t = np.arange(NLAT)
    row = (t // 64).astype(np.float32)
    col = (t % 64).astype(np.float32)
    nf = rot_dim // 4
    freqs = (np.float32(10000.0) ** (-np.arange(nf, dtype=np.float32) / np.float32(nf))).astype(np.float32)
    ang = np.concatenate([row[:, None] * freqs, col[:, None] * freqs], axis=-1).astype(np.float32)
    return np.cos(ang).astype(np.float32), np.sin(ang).astype(np.float32)


def _na_patterns():
    pats, plan, store = {}, [], []
    i = np.arange(128)
    for t in range(NTL):
        qr = 2 * t + i // 64
        qc = i % 64
        r0 = np.clip(qr - 4, 0, 56)
        c0 = np.clip(qc - 8, 0, 48)
        lst = []
        for u in range(NTL):
            kr = 2 * u + i // 64
            kc = i % 64
            ok = ((kr[:, None] >= r0[None, :]) & (kr[:, None] < r0[None, :] + 8)
                  & (kc[:, None] >= c0[None, :]) & (kc[:, None] < c0[None, :] + 16))
            if not ok.any():
                continue
            dr = np.clip(kr[:, None] - qr[None, :] + 7, 0, 14)
            dc = np.clip(kc[:, None] - qc[None, :], -15, 15) + 15
            key = (ok.tobytes(), (dr * ok).tobytes(), (dc * ok).tobytes())
            if key not in pats:
                pats[key] = len(store)
                store.append((dr, dc, ok))
            lst.append((u, pats[key]))
        plan.append(lst)
    dr = np.stack([s[0] for s in store])
    dc = np.stack([s[1] for s in store])
    ok = np.stack([s[2] for s in store])
    return plan, dr, dc, ok


_NA_PLAN, _NA_DR, _NA_DC, _NA_OK = _na_patterns()
NPAT = _NA_DR.shape[0]


class Builder:
    def __init__(self, depth=DEPTH, debug=False, stages=None, tile_list=None):
        self.tile_list = tile_list
        self.cut = 99
        self.depth = depth
        self.debug = debug
        self.stages = stages
        self.nc = bass.Bass("TRN2", target_bir_lowering=False)
        self.dram = {}

    def sbuf(self, name, shape, dt):
        self._uid = getattr(self, "_uid", 0) + 1
        return self.nc.sbuf_tensor(f"{name}_u{self._uid}", shape, dt)

    def psum(self, name, shape, dt):
        self._uid = getattr(self, "_uid", 0) + 1
        self.S.psum_keys.add(f"{name}_u{self._uid}")
        return self.nc.psum_tensor(f"{name}_u{self._uid}", shape, dt)

    def din(self, name, shape, dt=F32):
        self.dram[name] = self.nc.dram_tensor(name, list(shape), dt, kind="ExternalInput").ap()
        return self.dram[name]

    def dscr(self, name, shape, dt=F32):
        kind = "ExternalOutput" if self.debug else "Internal"
        self.dram[name] = self.nc.dram_tensor(name, list(shape), dt, kind=kind).ap()
        return self.dram[name]

    def declare(self):
        dp = self.depth
        self.din("x", [NLAT, D]); self.din("ctx", [NCTX, D])
        self.din("c_l", [128, 8]); self.din("cctx_l", [128, 8])
        self.din("ident", [128, 128])
        self.din("ada_w", [DEPTH, D, 6 * D]); self.din("ada_b", [DEPTH, 6 * D])
        self.din("norm1_g", [DEPTH, D]); self.din("norm2_g", [DEPTH, D])
        self.din("w_in", [DEPTH, D, INW]); self.din("w_out", [DEPTH, D, D])
        self.din("na_qn_g", [DEPTH, 64]); self.din("na_kn_g", [DEPTH, 64])
        self.din("na_bias", [DEPTH, NPAT, 4, 128, 128])
        self.din("gqa_qn_g", [DEPTH, 64]); self.din("gqa_kn_g", [DEPTH, 64])
        self.din("dn_conv_l", [DEPTH, 128, 6, 5])
        self.din("dn_a_log", [DEPTH, 8]); self.din("dn_dt_bias", [DEPTH, 8]); self.din("dn_out_g", [DEPTH, 64])
        self.din("mla_cq_g", [DEPTH, 256]); self.din("mla_ckv_g", [DEPTH, 128])
        self.din("mla_w_uq", [DEPTH, 256, 384]); self.din("mla_w_ukv", [DEPTH, 128, 512])
        self.din("mla_qn_g", [DEPTH, 96]); self.din("mla_kn_g", [DEPTH, 96])
        self.din("router_w", [DEPTH, D, NE]); self.din("router_b", [DEPTH, NE])
        if self.want("MOE"):
            self.din("exp_w1", [DEPTH, NE, D, 2 * D]); self.din("exp_b1_l", [DEPTH, 128, NE, 16])
            self.din("exp_w2", [DEPTH, NE, D, D]); self.din("exp_b2", [DEPTH, NE, D])
        self.din("cos_g", [NLAT, 32]); self.din("sin_g", [NLAT, 32])
        self.din("cos_m", [NLAT, 16]); self.din("sin_m", [NLAT, 16])
        self.din("dn_consts", [64, 7, 8, 64])
        self.din("sel65", [65, 64]); self.din("blk64", [128, 128])
        self.out = self.nc.dram_tensor("out", [NLAT, D], F32, kind="ExternalOutput").ap()
        self.dscr("modv", [2, 128, 6 * D])
        self.dscr("qkT_na", [8, 64, T], BF16); self.dscr("V_na", [T, 4 * 65], BF16)
        self.dscr("qkT_gq", [6, 64, T], BF16); self.dscr("V_gq", [T, 2 * 65], BF16)
        self.dscr("qT_ml", [4, 96, T], BF16); self.dscr("kT_ml", [4, 96, T], BF16); self.dscr("V_ml", [T, 4 * 65], BF16)
        self.dscr("dnraw", [768, T]); self.dscr("dn_gate", [T, 256]); self.dscr("dn_ba", [T, 16])
        self.dscr("dnT2", [12, 64, T]); self.dscr("o_dn", [2, T, 256])
        self.dscr("yT", [D, T], BF16)
        self.dscr("x1", [T, D]); self.dscr("h2T", [8, 128, T], BF16); self.dscr("combT", [NE, T])
        self.dscr("xs", [T, D])

    def build(self):
        nc = self.nc
        self.declare()
        with contextlib.ExitStack() as st:
            self.S = Sched(nc, st)
            self.idf = st.enter_context(self.sbuf("idf", [128, 128], F32))
            self.idb = st.enter_context(self.sbuf("idb", [128, 128], BF16))
            self.scB = st.enter_context(self.sbuf("scB", [128, 2, 8, 128], BF16))
            S = self.S
            S.dma("sp", self.idf[:], self.dram["ident"][:, :], writes=[self.idf])
            S.op("dve", lambda: nc.vector.tensor_copy(self.idb[:], self.idf[:]), [self.idf], [self.idb])
            self.stage_silu_c()
            for l in range(self.depth):
                last = (l == DEPTH - 1)
                xin = (self.dram["x"], self.dram["ctx"]) if l == 0 else (self.dram["xs"][0:NLAT], self.dram["xs"][NLAT:T])
                if self.want("S0"): self.stage_adaln(l)
                if self.want("SA"): self.stage_a(l, xin)
                if self.want("GQ"): self.stage_attn(l, "gq", with_ctx=not last)
                if self.want("ML"): self.stage_attn(l, "ml", with_ctx=not last)
                if self.want("NA"): self.stage_na(l, with_ctx=not last)
                if self.want("DN"): self.stage_dn(l, with_ctx=not last)
                if self.want("SO"): self.stage_out(l, xin, with_ctx=not last)
                if self.want("MOE"): self.stage_moe(l, with_ctx=not last)
            S.barrier()
            S.finish()
        return nc

    def want(self, s):
        return self.stages is None or s in self.stages

    def stage_silu_c(self):
        nc, S = self.nc, self.S
        with contextlib.ExitStack() as st:
            cl = st.enter_context(self.sbuf("cl", [128, 2, 8], F32))
            S.dma("sp", cl[:, 0, :], self.dram["c_l"][:, :], writes=[cl])
            S.dma("sp", cl[:, 1, :], self.dram["cctx_l"][:, :], writes=[cl])
            S.op("act", lambda: nc.scalar.activation(cl[:], cl[:], AF.Silu), [cl], [cl])
            for v in range(2):
                for k in range(8):
                    S.op("dve", lambda: nc.vector.tensor_copy(self.scB[:, v, k, :], cl[:, v, k:k + 1].to_broadcast([128, 128])), [cl], [self.scB])
            S.barrier()

    def stage_adaln(self, l):
        nc, S = self.nc, self.S
        with contextlib.ExitStack() as st:
            wa = [st.enter_context(self.sbuf(f"wa{i}", [128, 8, 512], BF16)) for i in range(2)]
            bb = [st.enter_context(self.sbuf(f"bb{i}", [128, 512], F32)) for i in range(2)]
            mo = [st.enter_context(self.sbuf(f"mo{i}", [128, 512], F32)) for i in range(2)]
            pm = [st.enter_context(self.psum(f"pm{i}", [128, 512], F32)) for i in range(2)]
            aw = self.dram["ada_w"][l].rearrange("(k p) n -> p k n", p=128)
            for n in range(12):
                w_, b_ = wa[n % 2], bb[n % 2]
                S.dma("pool", w_[:], aw[:, :, n * 512:(n + 1) * 512], writes=[w_])
                S.dma("sp", b_[:], self.dram["ada_b"][l, n * 512:(n + 1) * 512].partition_broadcast(128), writes=[b_])
                for v in range(2):
                    p_, m_ = pm[v], mo[v]
                    for k in range(8):
                        S.op("pe", lambda: nc.tensor.matmul(p_[:], self.scB[:, v, k, :], w_[:, k, :], start=(k == 0), stop=(k == 7)), [self.scB, w_], [p_])
                    S.op("dve", lambda: nc.vector.tensor_tensor(m_[:], p_[:], b_[:], ALU.add), [p_, b_], [m_])
                    S.dma("sp", self.dram["modv"][v, :, n * 512:(n + 1) * 512], m_[:], reads=[m_])
            S.barrier()

    def load_bcast(self, st, name, src_row_ap, n, q="sp"):
        t = st.enter_context(self.sbuf(name, [128, n], F32))
        self.S.dma(q, t[:], src_row_ap.partition_broadcast(128), writes=[t])
        return t

    def headnorm_k(self, src, src_keys, H, d, gain, out, sq, ss, tmp, _a=None, _b=None, _c=None, mean=True):
        nc, S = self.nc, self.S
        P = src.shape[0]
        S.op("act", lambda: nc.scalar.activation(sq, src, AF.Square), src_keys, [sq])
        S.op("dve", lambda: nc.vector.tensor_reduce(ss, sq, AX.X, ALU.add), [sq], [ss])
        S.op("act", lambda: nc.scalar.activation(ss, ss, AF.Sqrt, scale=(1.0 / d if mean else 1.0), bias=EPS), [ss], [ss])
        S.op("dve", lambda: nc.vector.reciprocal(ss, ss), [ss], [ss])
        bc = ss.unsqueeze(2).to_broadcast([P, H, d])
        if gain is None:
            S.op("dve", lambda: nc.vector.tensor_tensor(out, src, bc, ALU.mult), list(src_keys) + [ss], [out])
        else:
            S.op("dve", lambda: nc.vector.tensor_tensor(tmp, src, bc, ALU.mult), list(src_keys) + [ss], [tmp])
            S.op("pool", lambda: nc.gpsimd.tensor_tensor(out, tmp, gain, ALU.mult), [tmp, gain], [out])

    def rope(self, src, out, H, npair, cs, sn, ta, tb, src_k=None, out_k=None, cs_k=None, ta_k=None, tb_k=None):
        nc, S = self.nc, self.S
        x1, x2 = src[:, :, :, 0], src[:, :, :, 1]
        csb = cs.unsqueeze(1).to_broadcast([128, H, npair])
        snb = sn.unsqueeze(1).to_broadcast([128, H, npair])
        S.op("dve", lambda: nc.vector.tensor_tensor(ta, x1, csb, ALU.mult), [src_k, cs_k], [ta_k])
        S.op("pool", lambda: nc.gpsimd.tensor_tensor(tb, x2, snb, ALU.mult), [src_k, cs_k], [tb_k])
        S.op("dve", lambda: nc.vector.tensor_tensor(out[:, :, :, 0], ta, tb, ALU.subtract), [ta_k, tb_k], [out_k])
        S.op("dve", lambda: nc.vector.tensor_tensor(ta, x1, snb, ALU.mult), [src_k, cs_k], [ta_k])
        S.op("pool", lambda: nc.gpsimd.tensor_tensor(tb, x2, csb, ALU.mult), [src_k, cs_k], [tb_k])
        S.op("dve", lambda: nc.vector.tensor_tensor(out[:, :, :, 1], ta, tb, ALU.add), [ta_k, tb_k], [out_k])

    def stage_a(self, l, xin):
        nc, S, dr = self.nc, self.S, self.dram
        with contextlib.ExitStack() as st:
            sb = lambda name, shape, dt=F32: st.enter_context(self.sbuf(name, shape, dt))
            ps = lambda name, shape, dt=F32: st.enter_context(self.psum(name, shape, dt))
            win = sb("win", [128, 8, INW], BF16)
            wsrc = dr["w_in"][l].rearrange("(k p) n -> p k n", p=128)
            for k in range(0, 8, 2):
                S.dma("pool", win[:, k:k + 2, :], wsrc[:, k:k + 2, :], writes=[f"win{k}"])
            wink = [f"win{k - k % 2}" for k in range(8)]
            wuq = sb("wuq", [128, 2, 384], BF16)
            S.dma("pool", wuq[:], dr["mla_w_uq"][l].rearrange("(k p) n -> p k n", p=128), writes=[wuq])
            wukv = sb("wukv", [128, 512], BF16)
            S.dma("pool", wukv[:], dr["mla_w_ukv"][l], writes=[wukv])
            G, SH = [], []
            g1 = self.load_bcast(st, "g1", dr["norm1_g"][l], D)
            for v in range(2):
                Gv = sb(f"G{v}", [128, D]); Sv = sb(f"SH{v}", [128, D])
                S.dma("sp", Sv[:], dr["modv"][v, :, 0:D], writes=[Sv])
                S.dma("sp", Gv[:], dr["modv"][v, :, D:2 * D], writes=[Gv])
                S.op("dve", lambda: nc.vector.scalar_tensor_tensor(Gv[:], Gv[:], 1.0, g1[:], ALU.add, ALU.mult), [Gv, g1], [Gv])
                G.append(Gv); SH.append(Sv)
            gna = sb("gna", [128, 8, 64]); ggq = sb("ggq", [128, 6, 64]); gmq = sb("gmq", [128, 4, 96]); gmk = sb("gmk", [128, 4, 96])
            gt = self.load_bcast(st, "gt_naq", dr["na_qn_g"][l], 64)
            S.op("dve", lambda: nc.vector.tensor_scalar(gna[:, 0:4, :], gt[:].unsqueeze(1).to_broadcast([128, 4, 64]), 0.125, None, ALU.mult), [gt], [gna])
            gt = self.load_bcast(st, "gt_nak", dr["na_kn_g"][l], 64)
            S.op("dve", lambda: nc.vector.tensor_copy(gna[:, 4:8, :], gt[:].unsqueeze(1).to_broadcast([128, 4, 64])), [gt], [gna])
            gt = self.load_bcast(st, "gt_gqq", dr["gqa_qn_g"][l], 64)
            S.op("dve", lambda: nc.vector.tensor_scalar(ggq[:, 0:4, :], gt[:].unsqueeze(1).to_broadcast([128, 4, 64]), 0.125, None, ALU.mult), [gt], [ggq])
            gt = self.load_bcast(st, "gt_gqk", dr["gqa_kn_g"][l], 64)
            S.op("dve", lambda: nc.vector.tensor_copy(ggq[:, 4:6, :], gt[:].unsqueeze(1).to_broadcast([128, 2, 64])), [gt], [ggq])
            gt = self.load_bcast(st, "gt_mq", dr["mla_qn_g"][l], 96)
            S.op("dve", lambda: nc.vector.tensor_scalar(gmq[:], gt[:].unsqueeze(1).to_broadcast([128, 4, 96]), float(96 ** -0.5), None, ALU.mult), [gt], [gmq])
            gt = self.load_bcast(st, "gt_mk", dr["mla_kn_g"][l], 96)
            S.op("dve", lambda: nc.vector.tensor_copy(gmk[:], gt[:].unsqueeze(1).to_broadcast([128, 4, 96])), [gt], [gmk])
            gcq = self.load_bcast(st, "gcq", dr["mla_cq_g"][l], 256)
            gckv = self.load_bcast(st, "gckv", dr["mla_ckv_g"][l], 128)
            dbl = lambda name, shape, dt=F32: [sb(f"{name}_{i}", shape, dt) for i in range(2)]
            xt = dbl("xt", [128, D]); junks = dbl("junk", [128, D]); ssxs = dbl("ssx", [128, 1])
            hfs = dbl("hf", [128, D]); hbs = dbl("hb", [128, D], BF16); hTs = dbl("hT", [128, 8, 128], BF16)
            psbs = dbl("Tpsb", [128, INW]); dnTs = dbl("dnT", [128, 6, 128])
            sqs = dbl("sq", [128, 512]); sss = dbl("ss", [128, 8]); tmpns = dbl("tmpn", [128, 512])
            nbs = dbl("nb", [128, 8, 64], BF16); qkSs = dbl("qkS", [128, 8, 128], BF16)
            vaugs = dbl("vaug", [128, 4, 65], BF16); vaug2s = dbl("vaug2", [128, 2, 65], BF16); vaug3s = dbl("vaug3", [128, 4, 65], BF16)
            for va in vaugs + vaug2s + vaug3s:
                S.op("pool", lambda: nc.gpsimd.memset(va[:], 1.0), [], [va])
            gqfs = dbl("gqf", [128, 6, 64]); gqrs = dbl("gqr", [128, 6, 64], BF16)
            tas = dbl("ta", [128, 6 * 32]); tbs = dbl("tb", [128, 6 * 32]); rcs = dbl("rc", [128, 4, 32])
            cqns = dbl("cqn", [128, 256], BF16); cTs = dbl("cT", [128, 3, 128], BF16)
            mqs = dbl("mq", [128, 4, 96]); mqbs = dbl("mqb", [128, 4, 96], BF16)
            kfs = dbl("kf", [128, 4, 96]); mks = dbl("mk", [128, 4, 96]); mkbs = dbl("mkb", [128, 4, 96], BF16)
            pT = ps("pT", [128, 8, 128], BF16)
            pP = [ps(f"pP{i}", [128, 512]) for i in range(2)]
            pF = [ps(f"pF{i}", [128, 4, 128]) for i in range(2)]
            pQ = ps("pQ", [128, 8, 128], BF16)
            pM = ps("pM", [128, 512])
            chunks = [(0, 512), (512, 1024), (1024, 1280), (2048, 2560), (2560, 2736)]
            def body(t):
                par = t % 2
                junk, ssx, hf, hb, hT, psb, dnT = junks[par], ssxs[par], hfs[par], hbs[par], hTs[par], psbs[par], dnTs[par]
                sq, ss, tmpn, nb, qkS, vaug, vaug2, vaug3 = sqs[par], sss[par], tmpns[par], nbs[par], qkSs[par], vaugs[par], vaug2s[par], vaug3s[par]
                gqf, gqr, ta, tb, rc, cqn, cT = gqfs[par], gqrs[par], tas[par], tbs[par], rcs[par], cqns[par], cTs[par]
                mq, mqb, kf, mk, mkb = mqs[par], mqbs[par], kfs[par], mks[par], mkbs[par]
                rcg, rcm = "rcg%d" % par, "rcm%d" % par
                pk = lambda i: 'kpsb%d_%d' % (i, par)
                isc = t >= NTL
                v = 1 if isc else 0
                src = xin[1][(t - NTL) * 128:(t - NTL + 1) * 128, :] if isc else xin[0][t * 128:(t + 1) * 128, :]
                x_ = xt[t % 2]
                S.dma("sp", x_[:], src, writes=[x_])
                if not isc:
                    S.dma("act", rc[:, 0, :], dr["cos_g"][t * 128:(t + 1) * 128, :], writes=[rcg])
                    S.dma("act", rc[:, 1, :], dr["sin_g"][t * 128:(t + 1) * 128, :], writes=[rcg])
                    S.dma("act", rc[:, 2, 0:16], dr["cos_m"][t * 128:(t + 1) * 128, :], writes=[rcm])
                    S.dma("act", rc[:, 3, 0:16], dr["sin_m"][t * 128:(t + 1) * 128, :], writes=[rcm])
                S.op("act", lambda: nc.scalar.activation(junk[:], x_[:], AF.Square, accum_out=ssx[:]), [x_], [junk, ssx])
                S.op("act", lambda: nc.scalar.activation(ssx[:], ssx[:], AF.Sqrt, scale=1.0 / D, bias=EPS), [ssx], [ssx])
                S.op("dve", lambda: nc.vector.reciprocal(ssx[:], ssx[:]), [ssx], [ssx])
                S.op("dve", lambda: nc.vector.scalar_tensor_tensor(hf[:], x_[:], ssx[:, 0:1], G[v][:], ALU.mult, ALU.mult), [x_, ssx, G[v]], [hf])
                S.op("pool", lambda: nc.gpsimd.tensor_tensor(hb[:], hf[:], SH[v][:], ALU.add), [hf, SH[v]], [hb])
                yield
                for k in range(8):
                    S.op("pe", lambda: nc.tensor.transpose(pT[:, k, :], hb[:, k * 128:(k + 1) * 128], self.idb[:]), [hb, self.idb], [pT])
                S.op("act", lambda: nc.scalar.copy(hT[:], pT[:]), [pT], [hT])
                yield
                for ci, (a, b) in enumerate(chunks):
                    p_ = pP[ci % 2]
                    for k in range(8):
                        S.op("pe", lambda: nc.tensor.matmul(p_[:, 0:b - a], hT[:, k, :], win[:, k, a:b], start=(k == 0), stop=(k == 7)), [hT, wink[k]], [p_])
                    if ci % 2 == 0:
                        S.op("dve", lambda: nc.vector.tensor_copy(psb[:, a:b], p_[:, 0:b - a]), [p_], [pk(ci)])
                    else:
                        S.op("act", lambda: nc.scalar.copy(psb[:, a:b], p_[:, 0:b - a]), [p_], [pk(ci)])
                    yield
                for c in range(6):
                    pf = pF[c // 4]
                    for k in range(8):
                        S.op("pe", lambda: nc.tensor.matmul(pf[:, c % 4, :], win[:, k, 1280 + c * 128:1280 + (c + 1) * 128], hT[:, k, :], start=(k == 0), stop=(k == 7)), [hT, wink[k]], [pf])
                S.op("dve", lambda: nc.vector.tensor_copy(dnT[:, 0:4, :], pF[0][:]), [pF[0]], [dnT])
                S.op("act", lambda: nc.scalar.copy(dnT[:, 4:6, :], pF[1][:, 0:2, :]), [pF[1]], [dnT])
                yield
                S.dma("sp", dr["dnraw"].rearrange("(c p) n -> p c n", p=128)[:, :, t * 128:(t + 1) * 128], dnT[:], reads=[dnT])
                S.dma("sp", dr["dn_gate"][t * 128:(t + 1) * 128, :], psb[:, 2048:2304], reads=[pk(3)])
                S.dma("sp", dr["dn_ba"][t * 128:(t + 1) * 128, :], psb[:, 2304:2320], reads=[pk(3)])
                yield
                v3 = lambda ap, H: ap.rearrange("p (h d) -> p h d", h=H)
                self.headnorm_k(v3(psb[:, 0:512], 8), [pk(0)], 8, 64, gna[:], nb[:], v3(sq[:, 0:512], 8), ss[:, 0:8], v3(tmpn[:, 0:512], 8), [sq, ss, tmpn], gna, nb)
                yield
                for h in range(8):
                    S.op("pe", lambda: nc.tensor.transpose(pQ[0:64, h, :], nb[:, h, :], self.idb[:]), [nb, self.idb], [pQ])
                S.op("act", lambda: nc.scalar.copy(qkS[0:64, :, :], pQ[0:64, :, :]), [pQ], [qkS])
                yield
                S.dma("sp", dr["qkT_na"].rearrange("h d n -> d h n")[:, :, t * 128:(t + 1) * 128], qkS[0:64, :, :], reads=[qkS])
                S.op("pool", lambda: nc.gpsimd.tensor_copy(vaug[:, :, 0:64], v3(psb[:, 512:768], 4)), [pk(1)], [vaug])
                S.dma("sp", dr["V_na"][t * 128:(t + 1) * 128, :], vaug[:].rearrange("p h e -> p (h e)"), reads=[vaug])
                yield
                self.headnorm_k(v3(psb[:, 768:1152], 6), [pk(1), pk(2)], 6, 64, ggq[:], gqf[:], v3(sq[:, 0:384], 6), ss[:, 0:6], v3(tmpn[:, 0:384], 6), [sq, ss, tmpn], ggq, gqf)
                yield
                if isc:
                    S.op("dve", lambda: nc.vector.tensor_copy(gqr[:], gqf[:]), [gqf], [gqr])
                else:
                    v4 = lambda ap: ap.rearrange("p h (n two) -> p h n two", two=2)
                    self.rope(v4(gqf[:]), v4(gqr[:]), 6, 32, rc[:, 0, :], rc[:, 1, :], v3(ta[:], 6), v3(tb[:], 6), src_k=gqf, out_k=gqr, cs_k=rcg, ta_k=ta, tb_k=tb)
                for h in range(6):
                    S.op("pe", lambda: nc.tensor.transpose(pQ[0:64, h, :], gqr[:, h, :], self.idb[:]), [gqr, self.idb], [pQ])
                S.op("act", lambda: nc.scalar.copy(qkS[0:64, 0:6, :], pQ[0:64, 0:6, :]), [pQ], [qkS])
                yield
                S.dma("sp", dr["qkT_gq"].rearrange("h d n -> d h n")[:, :, t * 128:(t + 1) * 128], qkS[0:64, 0:6, :], reads=[qkS])
                S.op("pool", lambda: nc.gpsimd.tensor_copy(vaug2[:, :, 0:64], v3(psb[:, 1152:1280], 2)), [pk(2)], [vaug2])
                S.dma("sp", dr["V_gq"][t * 128:(t + 1) * 128, :], vaug2[:].rearrange("p h e -> p (h e)"), reads=[vaug2])
                yield
                v2 = lambda ap: ap.unsqueeze(1)
                self.headnorm_k(v2(psb[:, 2320:2576]), [pk(3), pk(4)], 1, 256, v2(gcq[:]), v2(cqn[:]), v2(sq[:, 0:256]), ss[:, 0:1], v2(tmpn[:, 0:256]), [sq, ss, tmpn], gcq, cqn)
                yield
                for k in range(2):
                    S.op("pe", lambda: nc.tensor.transpose(pT[:, k, :], cqn[:, k * 128:(k + 1) * 128], self.idb[:]), [cqn, self.idb], [pT])
                S.op("act", lambda: nc.scalar.copy(cT[:, 0:2, :], pT[:, 0:2, :]), [pT], [cT])
                yield
                for k in range(2):
                    S.op("pe", lambda: nc.tensor.matmul(pM[:, 0:384], cT[:, k, :], wuq[:, k, :], start=(k == 0), stop=(k == 1)), [cT, wuq], [pM])
                self.headnorm_k(v3(pM[:, 0:384], 4), [pM], 4, 96, gmq[:], mq[:], v3(sq[:, 0:384], 4), ss[:, 0:4], v3(tmpn[:, 0:384], 4), [sq, ss, tmpn], gmq, mq)
                yield
                S.op("pool", lambda: nc.gpsimd.tensor_copy(mqb[:], mq[:]), [mq], [mqb])
                if not isc:
                    v4m = lambda ap: ap[:, :, 64:96].rearrange("p h (n two) -> p h n two", two=2)
                    self.rope(v4m(mq[:]), v4m(mqb[:]), 4, 16, rc[:, 2, 0:16], rc[:, 3, 0:16], v3(ta[:, 0:64], 4), v3(tb[:, 0:64], 4), src_k=mq, out_k=mqb, cs_k=rcm, ta_k=ta, tb_k=tb)
                for h in range(4):
                    S.op("pe", lambda: nc.tensor.transpose(pQ[0:96, h, :], mqb[:, h, :], self.idb[:]), [mqb, self.idb], [pQ])
                S.op("act", lambda: nc.scalar.copy(qkS[0:96, 0:4, :], pQ[0:96, 0:4, :]), [pQ], [qkS])
                S.dma("sp", dr["qT_ml"].rearrange("h d n -> d h n")[:, :, t * 128:(t + 1) * 128], qkS[0:96, 0:4, :], reads=[qkS])
                yield
                self.headnorm_k(v2(psb[:, 2576:2704]), [pk(4)], 1, 128, v2(gckv[:]), v2(cqn[:, 0:128]), v2(sq[:, 0:128]), ss[:, 0:1], v2(tmpn[:, 0:128]), [sq, ss, tmpn], gckv, cqn)
                yield
                S.op("pe", lambda: nc.tensor.transpose(pT[:, 2, :], cqn[:, 0:128], self.idb[:]), [cqn, self.idb], [pT])
                S.op("act", lambda: nc.scalar.copy(cT[:, 2, :], pT[:, 2, :]), [pT], [cT])
                yield
                S.op("pe", lambda: nc.tensor.matmul(pM[:, :], cT[:, 2, :], wukv[:, :], start=True, stop=True), [cT, wukv], [pM])
                kv = pM[:, :].rearrange("p (h e) -> p h e", h=4)
                S.op("dve", lambda: nc.vector.tensor_copy(kf[:, :, 0:64], kv[:, :, 0:64]), [pM], [kf])
                S.op("dve", lambda: nc.vector.tensor_copy(kf[:, :, 64:96], psb[:, 2704:2736].unsqueeze(1).to_broadcast([128, 4, 32])), [pk(4)], [kf])
                S.op("dve", lambda: nc.vector.tensor_copy(vaug3[:, :, 0:64], kv[:, :, 64:128]), [pM], [vaug3])
                S.dma("sp", dr["V_ml"][t * 128:(t + 1) * 128, :], vaug3[:].rearrange("p h e -> p (h e)"), reads=[vaug3])
                yield
                self.headnorm_k(kf[:], [kf], 4, 96, gmk[:], mk[:], v3(sq[:, 0:384], 4), ss[:, 0:4], v3(tmpn[:, 0:384], 4), [sq, ss, tmpn], gmk, mk)
                yield
                S.op("pool", lambda: nc.gpsimd.tensor_copy(mkb[:], mk[:]), [mk], [mkb])
                if not isc:
                    self.rope(v4m(mk[:]), v4m(mkb[:]), 4, 16, rc[:, 2, 0:16], rc[:, 3, 0:16], v3(ta[:, 0:64], 4), v3(tb[:, 0:64], 4), src_k=mk, out_k=mkb, cs_k=rcm, ta_k=ta, tb_k=tb)
                for h in range(4):
                    S.op("pe", lambda: nc.tensor.transpose(pQ[0:96, h, :], mkb[:, h, :], self.idb[:]), [mkb, self.idb], [pQ])
                S.op("act", lambda: nc.scalar.copy(qkS[0:96, 0:4, :], pQ[0:96, 0:4, :]), [pQ], [qkS])
                S.dma("sp", dr["kT_ml"].rearrange("h d n -> d h n")[:, :, t * 128:(t + 1) * 128], qkS[0:96, 0:4, :], reads=[qkS])
            tiles = list(self.tile_list or range(NT))
            active, nxt = [], 0
            while nxt < len(tiles) or active:
                while len(active) < 2 and nxt < len(tiles):
                    active.append(body(tiles[nxt])); nxt += 1
                for g in list(active):
                    try:
                        next(g)
                    except StopIteration:
                        active.remove(g)
            S.barrier()

    def attn_core(self, st_tiles, kT, Vg, qT, dk, q0, qn, ktiles, yrow, bias_fn=None):
        nc, S, dr = self.nc, self.S, self.dram
        P, pS, pO, pD, Osb, rden, yb, sel65, sadd = st_tiles
        dkp = 128 if dk == 64 else dk
        self._blk = getattr(self, "_blk", 0) + 1
        po = pO[self._blk % 2]
        n = len(ktiles)
        for i in range(n + 1):
            if i < n:
                kt = ktiles[i]
                ps_ = pS[i % 2]
                S.op("pe", lambda: nc.tensor.matmul(ps_[:, 0:qn], kT[0:dkp, kt * 128:(kt + 1) * 128], qT[0:dkp, q0:q0 + qn], start=True, stop=True), [kT, qT], [ps_])
                p_ = P[i % 4]
                b = bias_fn(kt) if bias_fn else None
                if b is not None:
                    S.op("dve", lambda: nc.vector.tensor_tensor(sadd[:, 0:qn], ps_[:, 0:qn], b[0], ALU.add), [ps_, b[1]], [sadd])
                    S.op("act", lambda: nc.scalar.activation(p_[:, 0:qn], sadd[:, 0:qn], AF.Exp), [sadd], [p_])
                else:
                    S.op("act", lambda: nc.scalar.activation(p_[:, 0:qn], ps_[:, 0:qn], AF.Exp), [ps_], [p_])
            if i >= 1:
                j = i - 1
                kt = ktiles[j]
                p_ = P[j % 4]
                S.op("pe", lambda: nc.tensor.matmul(po[0:65, 0:qn], Vg[:, kt, :], p_[:, 0:qn], start=(j == 0), stop=(j == n - 1)), [Vg, p_], [po])
        S.op("act", lambda: nc.scalar.copy(Osb[:, 0:qn], po[0:65, 0:qn]), [po], [Osb])
        S.op("pe", lambda: nc.tensor.matmul(pD[0:64, 0:qn], sel65[:, :], Osb[:, 0:qn], start=True, stop=True), [sel65, Osb], [pD])
        S.op("dve", lambda: nc.vector.reciprocal(rden[:, 0:qn], pD[0:64, 0:qn]), [pD], [rden])
        S.op("pool", lambda: nc.gpsimd.tensor_tensor(yb[:, 0:qn], Osb[0:64, 0:qn], rden[:, 0:qn], ALU.mult), [Osb, rden], [yb])
        S.dma("sp", dr["yT"][yrow:yrow + 64, q0:q0 + qn], yb[:, 0:qn], reads=[yb])

    def attn_tiles(self, st):
        nc = self.nc
        sb = lambda name, shape, dt=F32: st.enter_context(self.sbuf(name, shape, dt))
        ps = lambda name, shape, dt=F32: st.enter_context(self.psum(name, shape, dt))
        P = [sb(f"P{i}", [128, 512], BF16) for i in range(4)]
        pS = [ps(f"pS{i}", [128, 512]) for i in range(2)]
        pO = [ps(f"pO{i}", [128, 512]) for i in range(2)]
        pD = ps("pD", [128, 512])
        Osb = sb("Osb", [65, 512]); rden = sb("rden", [64, 512]); yb = sb("yb", [64, 512], BF16)
        sel65 = sb("sel65_sb", [65, 64]); sadd = sb("sadd", [128, 512])
        self.S.dma("sp", sel65[:], self.dram["sel65"][:, :], writes=[sel65])
        return (P, pS, pO, pD, Osb, rden, yb, sel65, sadd)

    def stage_attn(self, l, kind, with_ctx):
        nc, S, dr = self.nc, self.S, self.dram
        if kind == "gq":
            qsrc, ksrc, V, dk, qpk, Hk, ybase = dr["qkT_gq"][0:4], dr["qkT_gq"][4:6], dr["V_gq"], 64, 2, 2, 256
        else:
            qsrc, ksrc, V, dk, qpk, Hk, ybase = dr["qT_ml"], dr["kT_ml"], dr["V_ml"], 96, 1, 4, 768
        with contextlib.ExitStack() as st:
            sb = lambda name, shape, dt=F32: st.enter_context(self.sbuf(name, shape, dt))
            tiles = self.attn_tiles(st)
            dkp = 128 if dk == 64 else dk
            kTs = [sb(f"kT{i}", [dkp, T], BF16) for i in range(2)]
            Vs = [sb(f"Vg{i}", [128, NT, 65], BF16) for i in range(2)]
            qTs = [sb(f"qT{i}", [dkp, T], BF16) for i in range(2)]
            if dkp != dk:
                for t_ in kTs + qTs:
                    S.op("pool", lambda: nc.gpsimd.memset(t_[dk:dkp, :], 0.0), [], [t_])
            blocks = [(b * 512, 512, list(range(NT))) for b in range(NLAT // 512)]
            if with_ctx:
                blocks.append((NLAT, NCTX, [NTL, NTL + 1]))
            hq = 0
            for g in range(Hk):
                kT, Vg = kTs[g % 2], Vs[g % 2]
                S.dma("sp", kT[0:dk, :], ksrc[g], writes=[kT])
                S.dma("act", Vg[:], V.rearrange("(n p) e -> p n e", p=128)[:, :, g * 65:(g + 1) * 65], writes=[Vg])
                for r in range(qpk):
                    h = g * qpk + r
                    qT = qTs[hq % 2]; hq += 1
                    S.dma("sp", qT[0:dk, :], qsrc[h], writes=[qT])
                    for (q0, qn, ktiles) in blocks:
                        self.attn_core(tiles, kT, Vg, qT, dk, q0, qn, ktiles, ybase + h * 64)
            S.barrier()

    def stage_na(self, l, with_ctx):
        nc, S, dr = self.nc, self.S, self.dram
        with contextlib.ExitStack() as st:
            sb = lambda name, shape, dt=F32: st.enter_context(self.sbuf(name, shape, dt))
            tiles = self.attn_tiles(st)
            kTn = sb("kTn", [128, 4, T], BF16); qTn = sb("qTn", [128, 4, T], BF16)
            for t_ in (kTn, qTn):
                S.op("pool", lambda: nc.gpsimd.memset(t_[64:128, :, :], 0.0), [], [t_])
            Vn = sb("Vn", [128, NT, 4 * 65], BF16)
            biasT = sb("biasT", [128, NPAT * 4, 128])
            S.dma("sp", kTn[0:64], dr["qkT_na"][4:8].rearrange("h d n -> d h n"), writes=[kTn])
            S.dma("sp", qTn[0:64], dr["qkT_na"][0:4].rearrange("h d n -> d h n"), writes=[qTn])
            S.dma("act", Vn[:], dr["V_na"].rearrange("(n p) e -> p n e", p=128), writes=[Vn])
            for pi in range(NPAT):
                S.dma("act", biasT[:, pi * 4:(pi + 1) * 4, :], dr["na_bias"][l, pi].rearrange("h j i -> j h i"), writes=[biasT])
            for t in range(NTL):
                plan = dict(_NA_PLAN[t])
                ktiles = sorted(plan.keys()) + [NTL, NTL + 1]
                for h in range(4):
                    bf = lambda kt: ((biasT[:, plan[kt] * 4 + h, :], biasT) if kt in plan else None)
                    self.attn_core(tiles, kTn[:, h, :], Vn[:, :, h * 65:(h + 1) * 65], qTn[:, h, :], 64, t * 128, 128, ktiles, h * 64, bias_fn=bf)
            if with_ctx:
                for h in range(4):
                    self.attn_core(tiles, kTn[:, h, :], Vn[:, :, h * 65:(h + 1) * 65], qTn[:, h, :], 64, NLAT, NCTX, [NTL, NTL + 1], h * 64)
            S.barrier()

    def stage_out(self, l, xin, with_ctx):
        nc, S, dr = self.nc, self.S, self.dram
        with contextlib.ExitStack() as st:
            sb = lambda name, shape, dt=F32: st.enter_context(self.sbuf(name, shape, dt))
            ps = lambda name, shape, dt=F32: st.enter_context(self.psum(name, shape, dt))
            wout = sb("wout", [128, 8, D], BF16)
            S.dma("pool", wout[:], dr["w_out"][l].rearrange("(k p) n -> p k n", p=128), writes=[wout])
            rw = sb("rw", [128, 8, NE])
            S.dma("sp", rw[:], dr["router_w"][l].rearrange("(k p) n -> p k n", p=128), writes=[rw])
            rb = self.load_bcast(st, "rb", dr["router_b"][l], NE)
            g2 = self.load_bcast(st, "g2", dr["norm2_g"][l], D)
            nv = 2 if with_ctx else 1
            GA, G2, SH2 = [], [], []
            for v in range(nv):
                ga = sb(f"GA{v}", [128, D]); Gv = sb(f"G2{v}", [128, D]); Sv = sb(f"SH2{v}", [128, D])
                S.dma("sp", ga[:], dr["modv"][v, :, 2 * D:3 * D], writes=[ga])
                S.dma("sp", Sv[:], dr["modv"][v, :, 3 * D:4 * D], writes=[Sv])
                S.dma("sp", Gv[:], dr["modv"][v, :, 4 * D:5 * D], writes=[Gv])
                S.op("dve", lambda: nc.vector.scalar_tensor_tensor(Gv[:], Gv[:], 1.0, g2[:], ALU.add, ALU.mult), [Gv, g2], [Gv])
                GA.append(ga); G2.append(Gv); SH2.append(Sv)
            yt = [sb(f"yt{i}", [128, 8, 128], BF16) for i in range(2)]
            xt = [sb(f"xo{i}", [128, D]) for i in range(2)]
            x1ts = [sb(f"x1t{i}", [128, D]) for i in range(2)]; tmps = [sb(f"tmpo{i}", [128, D]) for i in range(2)]; junks = [sb(f"junko{i}", [128, D]) for i in range(2)]
            ssx = sb("ssxo", [128, 1]); h2fs = [sb(f"h2f{i}", [128, D]) for i in range(2)]
            h2Tfs = [sb(f"h2Tf{i}", [128, 8, 128]) for i in range(2)]; h2Tbs = [sb(f"h2Tb{i}", [128, 8, 128], BF16) for i in range(2)]
            lg = sb("lg", [128, NE]); m8 = sb("m8", [128, 8]); nmx = sb("nmx", [128, 1]); ex = sb("ex", [128, NE])
            msk = sb("msk", [128, NE]); sm = sb("sm", [128, 1]); cmb = sb("cmb", [128, NE]); cmT = sb("cmT", [NE, 128])
            pP = [ps(f"pPo{i}", [128, 512]) for i in range(2)]
            pTt = [ps(f"pTt{i}", [128, 4, 128]) for i in range(2)]
            pL = ps("pL", [128, 512])
            ntile = NT if with_ctx else NTL
            for t in range(ntile):
                isc = t >= NTL
                v = 1 if isc else 0
                src = xin[1][(t - NTL) * 128:(t - NTL + 1) * 128, :] if isc else xin[0][t * 128:(t + 1) * 128, :]
                x_, y_ = xt[t % 2], yt[t % 2]
                x1t, tmp, junk, h2f, h2Tf, h2Tb = x1ts[t % 2], tmps[t % 2], junks[t % 2], h2fs[t % 2], h2Tfs[t % 2], h2Tbs[t % 2]
                tk = t % 2
                S.dma("sp", x_[:], src, writes=[x_])
                S.dma("act", y_[:], dr["yT"].rearrange("(k p) n -> p k n", p=128)[:, :, t * 128:(t + 1) * 128], writes=[y_])
                for n in range(2):
                    for k in range(8):
                        S.op("pe", lambda: nc.tensor.matmul(pP[n][:], y_[:, k, :], wout[:, k, n * 512:(n + 1) * 512], start=(k == 0), stop=(k == 7)), [y_, wout], [pP[n]])
                    S.op("dve", lambda: nc.vector.tensor_tensor(tmp[:, n * 512:(n + 1) * 512], pP[n][:], GA[v][:, n * 512:(n + 1) * 512], ALU.mult), [pP[n], GA[v]], [f"tmpk{n}_{tk}"])
                    S.op("pool", lambda: nc.gpsimd.tensor_tensor(x1t[:, n * 512:(n + 1) * 512], tmp[:, n * 512:(n + 1) * 512], x_[:, n * 512:(n + 1) * 512], ALU.add), [f"tmpk{n}_{tk}", x_], [f"x1k{n}_{tk}"])
                S.dma("sp", dr["x1"][t * 128:(t + 1) * 128, :], x1t[:], reads=[f"x1k0_{tk}", f"x1k1_{tk}"])
                if self.cut <= 1: continue
                S.op("act", lambda: nc.scalar.activation(junk[:], x1t[:], AF.Square, accum_out=ssx[:]), [f"x1k0_{tk}", f"x1k1_{tk}"], [junk, ssx])
                S.op("act", lambda: nc.scalar.activation(ssx[:], ssx[:], AF.Sqrt, scale=1.0 / D, bias=EPS), [ssx], [ssx])
                S.op("dve", lambda: nc.vector.reciprocal(ssx[:], ssx[:]), [ssx], [ssx])
                S.op("dve", lambda: nc.vector.scalar_tensor_tensor(junk[:], x1t[:], ssx[:, 0:1], G2[v][:], ALU.mult, ALU.mult), [f"x1k0_{tk}", f"x1k1_{tk}", ssx, G2[v]], [junk])
                S.op("pool", lambda: nc.gpsimd.tensor_tensor(h2f[:], junk[:], SH2[v][:], ALU.add), [junk, SH2[v]], [h2f])
                if self.cut <= 2: continue
                for k in range(8):
                    S.op("pe", lambda: nc.tensor.transpose(pTt[k // 4][:, k % 4, :], h2f[:, k * 128:(k + 1) * 128], self.idf[:]), [h2f, self.idf], [pTt[k // 4]])
                for hh in range(2):
                    S.op("act", lambda: nc.scalar.copy(h2Tf[:, hh * 4:(hh + 1) * 4, :], pTt[hh][:]), [pTt[hh]], [h2Tf])
                    S.op("dve", lambda: nc.vector.tensor_copy(h2Tb[:, hh * 4:(hh + 1) * 4, :], pTt[hh][:]), [pTt[hh]], [h2Tb])
                S.dma("sp", dr["h2T"].rearrange("k p n -> p k n")[:, :, t * 128:(t + 1) * 128], h2Tb[:], reads=[h2Tb])
                if self.cut <= 3: continue
                for k in range(8):
                    S.op("pe", lambda: nc.tensor.matmul(pL[:, 0:NE], h2Tf[:, k, :], rw[:, k, :], start=(k == 0), stop=(k == 7)), [h2Tf, rw], [pL])
                S.op("dve", lambda: nc.vector.tensor_tensor(lg[:], pL[:, 0:NE], rb[:], ALU.add), [pL, rb], [lg])
                if self.cut <= 4: continue
                S.op("dve", lambda: nc.vector.max(m8[:], lg[:]), [lg], [m8])
                S.op("dve", lambda: nc.vector.tensor_scalar(nmx[:], m8[:, 0:1], -1.0, None, ALU.mult), [m8], [nmx])
                S.op("act", lambda: nc.scalar.activation(ex[:], lg[:], AF.Exp, bias=nmx[:, 0:1], scale=1.0), [lg, nmx], [ex])
                S.op("dve", lambda: nc.vector.tensor_scalar(msk[:], lg[:], m8[:, 3:4], None, ALU.is_ge), [lg, m8], [msk])
                S.op("dve", lambda: nc.vector.tensor_tensor(ex[:], ex[:], msk[:], ALU.mult), [ex, msk], [ex])
                S.op("dve", lambda: nc.vector.tensor_reduce(sm[:], ex[:], AX.X, ALU.add), [ex], [sm])
                S.op("dve", lambda: nc.vector.reciprocal(sm[:], sm[:]), [sm], [sm])
                S.op("dve", lambda: nc.vector.tensor_scalar(cmb[:], ex[:], sm[:, 0:1], None, ALU.mult), [ex, sm], [cmb])
                if self.cut <= 5: continue
                S.op("pe", lambda: nc.tensor.transpose(pL[0:NE, 128:256], cmb[:], self.idf[:]), [cmb, self.idf], [pL])
                S.op("act", lambda: nc.scalar.copy(cmT[:], pL[0:NE, 128:256]), [pL], [cmT])
                S.dma("sp", dr["combT"][:, t * 128:(t + 1) * 128], cmT[:], reads=[cmT])
            S.barrier()

    def stage_moe(self, l, with_ctx):
        nc, S, dr = self.nc, self.S, self.dram
        ntile = NT if with_ctx else NTL
        sizes = [7, 7, 7, 7, 6] if with_ctx else [8, 8, 8, 8]
        last = (l == DEPTH - 1)
        with contextlib.ExitStack() as st:
            sb = lambda name, shape, dt=F32: st.enter_context(self.sbuf(name, shape, dt))
            ps = lambda name, shape, dt=F32: st.enter_context(self.psum(name, shape, dt))
            GMAX = 8 * 128
            h2g = sb("h2g", [128, 8, GMAX], BF16); acc = sb("acc", [128, 8, GMAX])
            W1 = [sb(f"W1_{i}", [128, 8, 2 * D], BF16) for i in range(2)]
            W2 = sb("W2", [128, 8, D], BF16)
            actT = [sb(f"actT{i}", [128, 8, 512], BF16) for i in range(2)]
            tr3 = [sb(f"tr3{i}", [128, 512]) for i in range(2)]
            tsg = [sb(f"tsg{i}", [128, 512]) for i in range(2)]
            tr1 = [sb(f"tr1{i}", [128, 512]) for i in range(2)]
            tr2 = [sb(f"tr2{i}", [128, 512]) for i in range(2)]
            tng = [sb(f"tng{i}", [128, 512]) for i in range(2)]
            b1m = sb("b1m", [128, NE, 16]); sgb = sb("sgb", [128, 1]); c14 = sb("c14", [128, 1])
            cB = [sb(f"cB{i}", [128, GMAX]) for i in range(2)]
            b1 = sb("b1", [128, NE, 16])
            b2b = sb("b2b", [NE, D], BF16); cTb = sb("cTb", [NE, GMAX], BF16)
            S.dma("sp", b1[:], dr["exp_b1_l"][l], writes=[b1])
            S.op("dve", lambda: nc.vector.tensor_scalar(b1m[:], b1[:], -1.0, 7.0, ALU.mult, ALU.add), [b1], [b1m])
            S.op("dve", lambda: nc.vector.memset(sgb[:], 1.702 * 7.0), [], [sgb])
            S.op("dve", lambda: nc.vector.memset(c14[:], 14.0), [], [c14])
            S.dma("pool", b2b[:], dr["exp_b2"][l], writes=[b2b])
            nv = 2 if with_ctx else 1
            G6 = []
            for v in range(nv):
                g6 = sb(f"G6{v}", [128, D])
                S.dma("sp", g6[:], dr["modv"][v, :, 5 * D:6 * D], writes=[g6])
                G6.append(g6)
            x1t = sb("x1m", [128, D]); tmpf = sb("tmpf", [128, D]); x2t = sb("x2m", [128, D])
            pg = [ps(f"pg{i}", [128, 512]) for i in range(2)]
            pl = [ps(f"pl{i}", [128, 512]) for i in range(2)]
            po = [ps(f"po{i}", [128, 512]) for i in range(2)]
            pF = [ps(f"pFm{i}", [128, 4, 128]) for i in range(2)]
            t0 = 0
            cnt = 0
            for gsz in sizes:
                g0, gn = t0 * 128, gsz * 128
                S.dma("sp", h2g[:, :, 0:gn], dr["h2T"].rearrange("k p n -> p k n")[:, :, g0:g0 + gn], writes=[h2g])
                S.dma("pool", cTb[:, 0:gn], dr["combT"][:, g0:g0 + gn], writes=[cTb])
                blocks = [(b0, min(512, gn - b0)) for b0 in range(0, gn, 512)]
                for e in range(NE):
                    w1 = W1[e % 2]
                    w1k = [f"{w1.name}h{k // 4}" for k in range(8)]
                    if not getattr(self, "moe_no_dma", False):
                        for hk in range(2):
                            S.dma("pool", w1[:, hk * 4:(hk + 1) * 4, :], dr["exp_w1"][l, e].rearrange("(k p) n -> p k n", p=128)[:, hk * 4:(hk + 1) * 4, :], writes=[f"{w1.name}h{hk}"])
                        S.dma("pool", W2[:], dr["exp_w2"][l, e].rearrange("(k p) n -> p k n", p=128), writes=[W2])
                    cb = cB[e % 2]
                    S.dma("sp", cb[:, 0:gn], dr["combT"][e, g0:g0 + gn].partition_broadcast(128), writes=[cb])
                    S.op("dve", lambda: nc.vector.tensor_scalar(cb[:, 0:gn], cb[:, 0:gn], -1.0, None, ALU.mult), [cb], [cb])
                    for (b0, bn) in blocks:
                        aT = actT[cnt % 2]; cnt += 1
                        for j in range(8):
                            i2 = j % 2
                            for k in range(8):
                                S.op("pe", lambda: nc.tensor.matmul(pg[i2][:, 0:bn], w1[:, k, j * 128:(j + 1) * 128], h2g[:, k, b0:b0 + bn], start=(k == 0), stop=(k == 7)), [w1k[k], h2g], [pg[i2]])
                            for k in range(8):
                                S.op("pe", lambda: nc.tensor.matmul(pl[i2][:, 0:bn], w1[:, k, D + j * 128:D + (j + 1) * 128], h2g[:, k, b0:b0 + bn], start=(k == 0), stop=(k == 7)), [w1k[k], h2g], [pl[i2]])
                            if getattr(self, "moe_pe_only", False): continue
                            r3, sg_, r1, r2, ng = tr3[i2], tsg[i2], tr1[i2], tr2[i2], tng[i2]
                            S.op("act", lambda: nc.scalar.activation(r3[:, 0:bn], pg[i2][:, 0:bn], AF.Relu, bias=b1m[:, e, j:j + 1], scale=-1.0), [pg[i2], b1m], [r3])
                            S.op("act", lambda: nc.scalar.activation(sg_[:, 0:bn], r3[:, 0:bn], AF.Sigmoid, bias=sgb[:, 0:1], scale=-1.702), [r3, sgb], [sg_])
                            S.op("act", lambda: nc.scalar.activation(r1[:, 0:bn], pl[i2][:, 0:bn], AF.Relu, bias=b1m[:, e, 8 + j:9 + j], scale=-1.0), [pl[i2], b1m], [r1])
                            S.op("act", lambda: nc.scalar.activation(r2[:, 0:bn], r1[:, 0:bn], AF.Relu, bias=c14[:, 0:1], scale=-1.0), [r1, c14], [r2])
                            S.op("dve", lambda: nc.vector.scalar_tensor_tensor(ng[:, 0:bn], r3[:, 0:bn], -7.0, sg_[:, 0:bn], ALU.add, ALU.mult), [r3, sg_], [ng])
                            S.op("dve", lambda: nc.vector.scalar_tensor_tensor(ng[:, 0:bn], r2[:, 0:bn], -6.0, ng[:, 0:bn], ALU.add, ALU.mult), [r2, ng], [ng])
                            S.op("dve", lambda: nc.vector.tensor_tensor(aT[:, j, 0:bn], ng[:, 0:bn], cb[:, b0:b0 + bn], ALU.mult), [ng, cb], [f"{aT.name}j{j}"])
                        for c in range(8):
                            p_ = po[c % 2]
                            if e == 0:
                                S.op("pe", lambda: nc.tensor.matmul(p_[:, 0:bn], b2b[:, c * 128:(c + 1) * 128], cTb[:, b0:b0 + bn], start=True, stop=False), [b2b, cTb], [p_])
                            for j in range(8):
                                S.op("pe", lambda: nc.tensor.matmul(p_[:, 0:bn], W2[:, j, c * 128:(c + 1) * 128], aT[:, j, 0:bn], start=(j == 0 and e != 0), stop=(j == 7)), [W2, f"{aT.name}j{j}"], [p_])
                            if getattr(self, "moe_pe_only", False): continue
                            if e == 0:
                                S.op("dve", lambda: nc.vector.tensor_copy(acc[:, c, b0:b0 + bn], p_[:, 0:bn]), [p_], [f"acc{c}"])
                            else:
                                S.op("dve", lambda: nc.vector.tensor_tensor(acc[:, c, b0:b0 + bn], acc[:, c, b0:b0 + bn], p_[:, 0:bn], ALU.add), [p_, f"acc{c}"], [f"acc{c}"])
                for ti in range(gsz):
                    t = t0 + ti
                    isc = t >= NTL
                    v = 1 if isc else 0
                    S.dma("sp", x1t[:], dr["x1"][t * 128:(t + 1) * 128, :], writes=[x1t])
                    for c in range(8):
                        S.op("pe", lambda: nc.tensor.transpose(pF[c // 4][:, c % 4, :], acc[:, c, ti * 128:(ti + 1) * 128], self.idf[:]), [f"acc{c}", self.idf], [pF[c // 4]])
                    for hh in range(2):
                        S.op("dve", lambda: nc.vector.tensor_tensor(tmpf[:, hh * 512:(hh + 1) * 512], pF[hh][:].rearrange("p a b -> p (a b)"), G6[v][:, hh * 512:(hh + 1) * 512], ALU.mult), [pF[hh], G6[v]], [f"tmpf{hh}"])
                        S.op("dve", lambda: nc.vector.tensor_tensor(x2t[:, hh * 512:(hh + 1) * 512], tmpf[:, hh * 512:(hh + 1) * 512], x1t[:, hh * 512:(hh + 1) * 512], ALU.add), [f"tmpf{hh}", x1t], [x2t])
                    if last:
                        S.dma("sp", self.out[t * 128:(t + 1) * 128, :], x2t[:], reads=[x2t], is_output=True)
                    else:
                        S.dma("sp", dr["xs"][t * 128:(t + 1) * 128, :], x2t[:], reads=[x2t])
                t0 += gsz
            S.barrier()

    def stage_dn(self, l, with_ctx):
        nc, S, dr = self.nc, self.S, self.dram
        NCH = T // 64
        with contextlib.ExitStack() as st:
            sb = lambda name, shape, dt=F32: st.enter_context(self.sbuf(name, shape, dt))
            ps = lambda name, shape, dt=F32: st.enter_context(self.psum(name, shape, dt))
            cw = sb("cw", [128, 6, 5]); blk = sb("blk64", [128, 128])
            S.dma("sp", cw[:], dr["dn_conv_l"][l], writes=[cw])
            S.dma("sp", blk[:], dr["blk64"][:, :], writes=[blk])
            xr = [sb(f"xr{i}", [128, T]) for i in range(2)]
            xc = [sb(f"xc{i}", [128, T]) for i in range(2)]
            sqt = [sb(f"sqt{i}", [128, 512]) for i in range(2)]
            rs = [sb(f"rs{i}", [128, 512]) for i in range(2)]
            pn = [ps(f"pn{i}", [128, 512]) for i in range(2)]
            bi = 0
            for c in range(6):
                r_, c_ = xr[c % 2], xc[c % 2]
                S.dma("sp", r_[:], dr["dnraw"][c * 128:(c + 1) * 128, :], writes=[r_])
                for (a, b) in ((0, NLAT), (NLAT, T)):
                    S.op("dve", lambda: nc.vector.tensor_scalar(c_[:, a:b], r_[:, a:b], cw[:, c, 2:3], None, ALU.mult), [r_, cw], [c_])
                    for s_ in (0, 1, 3, 4):
                        off = s_ - 2
                        lo, hi = a + max(0, -off), b - max(0, off)
                        S.op("dve", lambda: nc.vector.scalar_tensor_tensor(c_[:, lo:hi], r_[:, lo + off:hi + off], cw[:, c, s_:s_ + 1], c_[:, lo:hi], ALU.mult, ALU.add), [r_, cw, c_], [c_])
                S.op("act", lambda: nc.scalar.activation(c_[:], c_[:], AF.Silu), [c_], [c_])
                if c < 4:
                    for b0 in range(0, T, 512):
                        bn = min(512, T - b0)
                        q_, r2, p_ = sqt[bi % 2], rs[bi % 2], pn[bi % 2]; bi += 1
                        S.op("act", lambda: nc.scalar.activation(q_[:, 0:bn], c_[:, b0:b0 + bn], AF.Square), [c_], [q_])
                        S.op("pe", lambda: nc.tensor.matmul(p_[:, 0:bn], blk[:], q_[:, 0:bn], start=True, stop=True), [blk, q_], [p_])
                        S.op("act", lambda: nc.scalar.activation(r2[:, 0:bn], p_[:, 0:bn], AF.Sqrt, scale=1.0, bias=EPS), [p_], [r2])
                        S.op("dve", lambda: nc.vector.reciprocal(r2[:, 0:bn], r2[:, 0:bn]), [r2], [r2])
                        S.op("dve", lambda: nc.vector.scalar_tensor_tensor(c_[:, b0:b0 + bn], c_[:, b0:b0 + bn], (0.125 if c < 2 else 1.0), r2[:, 0:bn], ALU.mult, ALU.mult), [c_, r2], [c_])
                S.dma("sp", dr["dnT2"].rearrange("h d n -> (h d) n")[c * 128:(c + 1) * 128, :], c_[:], reads=[c_])
            S.barrier()
        with contextlib.ExitStack() as st:
            sb = lambda name, shape, dt=F32: st.enter_context(self.sbuf(name, shape, dt))
            ps = lambda name, shape, dt=F32: st.enter_context(self.psum(name, shape, dt))
            dc = sb("dc", [64, 7, 8, 64])
            S.dma("sp", dc[:], dr["dn_consts"][:, :, :, :], writes=[dc])
            C_tri, C_mT, C_mN, C_sT, C_sN, C_id = (dc[:, i] for i in range(6))
            ones64 = dc[:, 6, 0, :]
            ba = sb("ba", [64, NCH, 16])
            S.dma("sp", ba[:], dr["dn_ba"].rearrange("(n c) e -> c n e", c=64), writes=[ba])
            alog = sb("alog", [64, 8]); dtb = sb("dtb", [64, 8])
            S.dma("sp", alog[:], dr["dn_a_log"][l].partition_broadcast(64), writes=[alog])
            S.dma("sp", dtb[:], dr["dn_dt_bias"][l].partition_broadcast(64), writes=[dtb])
            Q = sb("Qs", [64, NCH, 8, 6])
            z = sb("z", [64, NCH, 8]); az = sb("az", [64, NCH, 8]); t3 = sb("t3", [64, NCH, 8])
            pb = [ps(f"pb{i}", [64, 8, 64]) for i in range(8)]
            pgA, pgB = pb[0][:].rearrange("p a c -> p (a c)"), pb[1][:].rearrange("p a c -> p (a c)")
            pgL = [pb[2][:].rearrange("p a c -> p (a c)"), pb[3][:].rearrange("p a c -> p (a c)")]
            bcn = lambda t_: t_[:].unsqueeze(1).to_broadcast([64, NCH, 8])
            S.op("act", lambda: nc.scalar.activation(alog[:], alog[:], AF.Exp), [alog], [alog])
            S.op("dve", lambda: nc.vector.tensor_scalar(alog[:], alog[:], -1.0, None, ALU.mult), [alog], [alog])
            S.op("act", lambda: nc.scalar.activation(Q[:, :, :, 0], ba[:, :, 0:8], AF.Sigmoid), [ba], ["Q0"])
            S.op("dve", lambda: nc.vector.tensor_tensor(z[:], ba[:, :, 8:16], bcn(dtb), ALU.add), [ba, dtb], [z])
            S.op("act", lambda: nc.scalar.activation(az[:], z[:], AF.Abs), [z], [az])
            S.op("act", lambda: nc.scalar.activation(az[:], az[:], AF.Exp, scale=-1.0), [az], [az])
            S.op("act", lambda: nc.scalar.activation(az[:], az[:], AF.Ln, bias=1.0, scale=1.0), [az], [az])
            S.op("dve", lambda: nc.vector.tensor_scalar(z[:], z[:], 0.0, None, ALU.max), [z], [z])
            S.op("dve", lambda: nc.vector.tensor_tensor(z[:], z[:], az[:], ALU.add), [z, az], [z])
            S.op("dve", lambda: nc.vector.tensor_tensor(Q[:, :, :, 5], z[:], bcn(alog), ALU.mult), [z, alog], ["Q5"])
            S.op("dve", lambda: nc.vector.tensor_copy(z[:], Q[:, :, :, 5]), ["Q5"], [z])
            S.op("pe", lambda: nc.tensor.matmul(pgA[:, 0:NCH * 4].rearrange("p (n h) -> p n h", h=4), dc[:, 0, 0, :], z[:, :, 0:4], start=True, stop=True), [dc, z], [pgA])
            S.op("pe", lambda: nc.tensor.matmul(pgB[:, 0:NCH * 4].rearrange("p (n h) -> p n h", h=4), dc[:, 0, 4, :], z[:, :, 4:8], start=True, stop=True), [dc, z], [pgB])
            S.op("dve", lambda: nc.vector.tensor_copy(Q[:, :, 0:4, 1], pgA[:, 0:NCH * 4].rearrange("p (n h) -> p n h", h=4)), [pgA], ["Q1"])
            S.op("dve", lambda: nc.vector.tensor_copy(Q[:, :, 4:8, 1], pgB[:, 0:NCH * 4].rearrange("p (n h) -> p n h", h=4)), [pgB], ["Q1"])
            hf_ = NCH // 2
            for i in range(2):
                S.op("pe", lambda: nc.tensor.matmul(pgL[i][:, 0:hf_ * 8].rearrange("p (n h) -> p n h", h=8), ones64, z[:, i * hf_:(i + 1) * hf_, :], start=True, stop=True), [dc, z], [pgL[i]])
                gl = pgL[i][:, 0:hf_ * 8].rearrange("p (n h) -> p n h", h=8)
                sl = slice(i * hf_, (i + 1) * hf_)
                S.op("dve", lambda: nc.vector.tensor_tensor(t3[:, sl, :], gl, Q[:, sl, :, 1], ALU.subtract), [pgL[i], "Q1"], [t3])
                S.op("act", lambda: nc.scalar.activation(Q[:, sl, :, 4], gl, AF.Exp), [pgL[i]], ["Q4"])
            S.op("act", lambda: nc.scalar.activation(Q[:, :, :, 3], t3[:], AF.Exp), [t3], ["Q3"])
            S.op("act", lambda: nc.scalar.activation(az[:], Q[:, :, :, 1], AF.Exp), ["Q1"], [az])
            S.op("dve", lambda: nc.vector.tensor_tensor(Q[:, :, :, 2], az[:], Q[:, :, :, 0], ALU.mult), [az, "Q0"], ["Q2"])
            QK = ["Q0", "Q1", "Q2", "Q3", "Q4", "Q5"]
            T8 = lambda name: sb(name, [64, 8, 64])
            Dq = [sb(f"Dq{i}", [64, 2, 12, 64]) for i in range(2)]
            SSt = sb("SSt", [64, 8, 6])
            X, Dn, EG, decT, decN, dgb, t1, t2 = (T8(n) for n in ("X", "Dn", "EG", "decT", "decN", "dgb", "t1", "t2"))
            T8b = lambda name: sb(name, [64, 8, 64], BF16)
            Bm = [T8b("Bm0"), T8b("Bm1")]; Am = [T8b("Am0"), T8b("Am1")]
            attnT, kdec, u, wT, qgT, vnew, St, osb = (T8(n) for n in ("attnT", "kdec", "u", "wT", "qgT", "vnew", "St", "osb"))
            Pm, vb, kbg = T8b("Pm"), T8b("vb"), T8b("kbg")
            S.op("pool", lambda: nc.gpsimd.memset(St[:], 0.0), [], [St])
            Fw = list(range(NLAT // 64, NCH)) + list(range(NLAT // 64))
            Bw = list(range(NCH - 1, NLAT // 64 - 1, -1)) + list(range(NLAT // 64 - 1, -1, -1))
            v4 = lambda ap: ap.rearrange("p (a b) c -> p a b c", a=2)
            f2 = lambda ap: ap.rearrange("p a c -> p (a c)")
            idf64 = self.idf[0:64, 0:64]
            dsrc = dr["dnT2"].rearrange("h d n -> d h n")
            for s_ in range(NCH):
                f, b = Fw[s_], Bw[s_]
                D_ = Dq[s_ % 2]
                S.dma("sp", D_[:, 0], dsrc[:, :, f * 64:(f + 1) * 64], writes=[D_])
                S.dma("act", D_[:, 1], dsrc[:, :, b * 64:(b + 1) * 64], writes=[D_])
                S.op("pool", lambda: nc.gpsimd.tensor_copy(SSt[:, 0:4, :], Q[:, f, 0:4, :]), QK, [SSt])
                S.op("pool", lambda: nc.gpsimd.tensor_copy(SSt[:, 4:8, :], Q[:, b, 4:8, :]), QK, [SSt])
                bc = lambda q: SSt[:, :, q].unsqueeze(2).to_broadcast([64, 8, 64])
                qTs, kTs, vTs = D_[:, :, 0:4, :], D_[:, :, 4:8, :], D_[:, :, 8:12, :]
                slot = lambda base, i: D_[:, i // 4, base + i % 4, :]
                pA, pB, pK, pV, pKK, pKQ, pP_, pX = pb
                S.op("dve", lambda: nc.vector.tensor_tensor(X[:], C_tri, bc(5), ALU.mult), [dc, SSt], [X])
                S.op("pe", lambda: nc.tensor.matmul(f2(pA[:]), ones64, f2(X[:]), start=True, stop=True), [dc, X], [pA])
                S.op("dve", lambda: nc.vector.tensor_tensor(Dn[:], pA[:], bc(1), ALU.subtract), [pA, SSt], [Dn])
                S.op("act", lambda: nc.scalar.activation(EG[:], pA[:], AF.Exp), [pA], [EG])
                S.op("dve", lambda: nc.vector.tensor_tensor(decT[:], Dn[:], C_mT, ALU.add), [Dn, dc], [decT])
                S.op("act", lambda: nc.scalar.activation(decT[:], decT[:], AF.Exp), [decT], [decT])
                S.op("dve", lambda: nc.vector.scalar_tensor_tensor(decN[:], Dn[:], -1.0, C_mN, ALU.mult, ALU.add), [Dn, dc], [decN])
                S.op("act", lambda: nc.scalar.activation(decN[:], decN[:], AF.Exp), [decN], [decN])
                S.op("pool", lambda: nc.gpsimd.tensor_tensor(dgb[:], C_id, bc(0), ALU.mult), [dc, SSt], [dgb])
                S.op("pe", lambda: nc.tensor.matmul(f2(pB[:]), ones64, f2(dgb[:]), start=True, stop=True), [dc, dgb], [pB])
                for i in range(8):
                    S.op("pe", lambda: nc.tensor.transpose(pK[:, i, :], slot(4, i), idf64), [D_, self.idf], [pK])
                for i in range(8):
                    S.op("pe", lambda: nc.tensor.transpose(pV[:, i, :], slot(8, i), idf64), [D_, self.idf], [pV])
                for i in range(8):
                    S.op("pe", lambda: nc.tensor.matmul(pKK[:, i, :], slot(4, i), slot(4, i), start=True, stop=True), [D_], [pKK])
                for i in range(8):
                    S.op("pe", lambda: nc.tensor.matmul(pKQ[:, i, :], slot(4, i), slot(0, i), start=True, stop=True), [D_], [pKQ])
                S.op("pool", lambda: nc.gpsimd.tensor_tensor(t1[:], decT[:], C_sT, ALU.mult), [decT, dc], [t1])
                S.op("dve", lambda: nc.vector.tensor_tensor(t1[:], t1[:], pB[:], ALU.mult), [t1, pB], [t1])
                S.op("dve", lambda: nc.vector.tensor_tensor(Bm[0][:], t1[:], pKK[:], ALU.mult), [t1, pKK], [Bm[0]])
                S.op("pool", lambda: nc.gpsimd.tensor_tensor(t2[:], decN[:], C_sN, ALU.mult), [decN, dc], [t2])
                S.op("pool", lambda: nc.gpsimd.tensor_tensor(t2[:], t2[:], bc(0), ALU.mult), [t2, SSt], [t2])
                S.op("dve", lambda: nc.vector.tensor_tensor(Am[0][:], t2[:], pKK[:], ALU.mult), [t2, pKK], [Am[0]])
                S.op("dve", lambda: nc.vector.tensor_tensor(attnT[:], pKQ[:], decT[:], ALU.mult), [pKQ, decT], [attnT])
                S.op("pool", lambda: nc.gpsimd.tensor_tensor(Pm[:], C_id, Bm[0][:], ALU.subtract), [dc, Bm[0]], [Pm])
                cur = 0
                for r in range(1, 6):
                    nx = 1 - cur
                    if r < 5:
                        for i in range(8):
                            S.op("pe", lambda: nc.tensor.matmul(pA[:, i, :], Am[cur][:, i, :], Bm[cur][:, i, :], start=True, stop=True), [Am[cur], Bm[cur]], [pA])
                    for i in range(8):
                        S.op("pe", lambda: nc.tensor.matmul(pB[:, i, :], Bm[cur][:, i, :], Am[cur][:, i, :], start=True, stop=True), [Am[cur], Bm[cur]], [pB])
                    if r < 5:
                        S.op("act", lambda: nc.scalar.copy(Bm[nx][:], pA[:]), [pA], [Bm[nx]])
                    S.op("dve", lambda: nc.vector.tensor_copy(Am[nx][:], pB[:]), [pB], [Am[nx]])
                    for i in range(8):
                        S.op("pe", lambda: nc.tensor.matmul(pP_[:, i, :], Am[nx][:, i, :], Pm[:, i, :], start=True, stop=True), [Am[nx], Pm], [pP_])
                    S.op("dve", lambda: nc.vector.tensor_tensor(Pm[:], Pm[:], pP_[:], ALU.add), [Pm, pP_], [Pm])
                    cur = nx
                S.op("dve", lambda: nc.vector.tensor_tensor(vb[:], pV[:], bc(0), ALU.mult), [pV, SSt], [vb])
                S.op("dve", lambda: nc.vector.tensor_tensor(kbg[:], pK[:], bc(2), ALU.mult), [pK, SSt], [kbg])
                S.op("dve", lambda: nc.vector.tensor_tensor(kdec[:], pK[:], bc(3), ALU.mult), [pK, SSt], [kdec])
                for i in range(8):
                    S.op("pe", lambda: nc.tensor.matmul(pKK[:, i, :], Pm[:, i, :], vb[:, i, :], start=True, stop=True), [Pm, vb], [pKK])
                S.op("act", lambda: nc.scalar.copy(u[:], pKK[:]), [pKK], [u])
                for i in range(8):
                    S.op("pe", lambda: nc.tensor.matmul(pKQ[:, i, :], kbg[:, i, :], Pm[:, i, :], start=True, stop=True), [Pm, kbg], [pKQ])
                S.op("act", lambda: nc.scalar.copy(wT[:], pKQ[:]), [pKQ], [wT])
                S.op("pool", lambda: nc.gpsimd.tensor_tensor(v4(qgT[:]), qTs, v4(EG[:]), ALU.mult), [D_, EG], [qgT])
                for i in range(8):
                    S.op("pe", lambda: nc.tensor.matmul(pKK[:, i, :], wT[:, i, :], St[:, i, :], start=True, stop=True), [wT, St], [pKK])
                S.op("dve", lambda: nc.vector.tensor_tensor(vnew[:], u[:], pKK[:], ALU.subtract), [u, pKK], [vnew])
                for i in range(8):
                    S.op("pe", lambda: nc.tensor.matmul(pKQ[:, i, :], qgT[:, i, :], St[:, i, :], start=True, stop=False), [qgT, St], [pKQ])
                    S.op("pe", lambda: nc.tensor.matmul(pKQ[:, i, :], attnT[:, i, :], vnew[:, i, :], start=False, stop=True), [attnT, vnew], [pKQ])
                for i in range(8):
                    S.op("pe", lambda: nc.tensor.matmul(pP_[:, i, :], kdec[:, i, :], vnew[:, i, :], start=True, stop=True), [kdec, vnew], [pP_])
                S.op("pool", lambda: nc.gpsimd.tensor_tensor(St[:], St[:], bc(4), ALU.mult), [St, SSt], [St])
                S.op("dve", lambda: nc.vector.tensor_tensor(St[:], St[:], pP_[:], ALU.add), [St, pP_], [St])
                S.op("act", lambda: nc.scalar.copy(osb[:], pKQ[:]), [pKQ], [osb])
                S.dma("sp", dr["o_dn"][0, f * 64:(f + 1) * 64, :].rearrange("p (h e) -> p h e", h=4), osb[:, 0:4, :], reads=[osb])
                S.dma("sp", dr["o_dn"][1, b * 64:(b + 1) * 64, :].rearrange("p (h e) -> p h e", h=4), osb[:, 4:8, :], reads=[osb])
            S.barrier()
        with contextlib.ExitStack() as st:
            sb = lambda name, shape, dt=F32: st.enter_context(self.sbuf(name, shape, dt))
            ps = lambda name, shape, dt=F32: st.enter_context(self.psum(name, shape, dt))
            gd = sb("gdn", [128, 4, 64])
            gt = self.load_bcast(st, "gt_dn", dr["dn_out_g"][l], 64)
            S.op("dve", lambda: nc.vector.tensor_copy(gd[:], gt[:].unsqueeze(1).to_broadcast([128, 4, 64])), [gt], [gd])
            of = [sb(f"of{i}", [128, 256]) for i in range(2)]; ob = [sb(f"ob{i}", [128, 256]) for i in range(2)]
            gte = [sb(f"gte{i}", [128, 256]) for i in range(2)]
            sq = sb("sqd", [128, 256]); ss = sb("ssd", [128, 4]); tmpn = sb("tmpd", [128, 256]); yn = sb("ynd", [128, 256])
            ybf = sb("ybf", [128, 256], BF16); ySb = sb("ySb", [128, 2, 128], BF16)
            pT = ps("pTd", [128, 8, 128], BF16)
            v3 = lambda ap: ap.rearrange("p (h d) -> p h d", h=4)
            for t in range(NT if with_ctx else NTL):
                a_, b_, g_ = of[t % 2], ob[t % 2], gte[t % 2]
                S.dma("sp", a_[:], dr["o_dn"][0, t * 128:(t + 1) * 128, :], writes=[a_])
                S.dma("act", b_[:], dr["o_dn"][1, t * 128:(t + 1) * 128, :], writes=[b_])
                S.dma("sp", g_[:], dr["dn_gate"][t * 128:(t + 1) * 128, :], writes=[g_])
                S.op("pool", lambda: nc.gpsimd.tensor_tensor(a_[:], a_[:], b_[:], ALU.add), [a_, b_], [a_])
                self.headnorm_k(v3(a_[:]), [a_], 4, 64, gd[:], v3(yn[:]), v3(sq[:]), ss[:], v3(tmpn[:]))
                S.op("act", lambda: nc.scalar.activation(g_[:], g_[:], AF.Silu), [g_], [g_])
                S.op("dve", lambda: nc.vector.tensor_tensor(ybf[:], yn[:], g_[:], ALU.mult), [yn, g_], [ybf])
                for k in range(2):
                    S.op("pe", lambda: nc.tensor.transpose(pT[:, k, :], ybf[:, k * 128:(k + 1) * 128], self.idb[:]), [ybf, self.idb], [pT])
                S.op("act", lambda: nc.scalar.copy(ySb[:], pT[:, 0:2, :]), [pT], [ySb])
                S.dma("sp", dr["yT"][512:768, :].rearrange("(k p) n -> p k n", p=128)[:, :, t * 128:(t + 1) * 128], ySb[:], reads=[ySb])
            S.barrier()


def _dn_consts():
    t = np.arange(64)
    c = np.zeros((64, 7, 8, 64), np.float32)
    le = (t[:, None] <= t[None, :]).astype(np.float32)
    ge = (t[:, None] >= t[None, :]).astype(np.float32)
    lt = (t[:, None] < t[None, :]).astype(np.float32)
    gt = (t[:, None] > t[None, :]).astype(np.float32)
    eye = np.eye(64, dtype=np.float32)
    for s in range(8):
        fwd = s < 4
        c[:, 0, s, :] = le if fwd else ge
        c[:, 1, s, :] = (1 - (le if fwd else ge)) * MASKNEG
        c[:, 2, s, :] = (1 - (ge if fwd else le)) * MASKNEG
        c[:, 3, s, :] = lt if fwd else gt
        c[:, 4, s, :] = gt if fwd else lt
        c[:, 5, s, :] = eye
        c[:, 6, s, :] = 1.0
    return c


def _shared_inputs(inp):
    f = lambda a: np.ascontiguousarray(np.asarray(a, dtype=np.float32))
    sh = {}
    for k in ("ada_w", "ada_b", "norm1_g", "norm2_g", "w_in", "w_out", "na_qn_g", "na_kn_g", "gqa_qn_g", "gqa_kn_g",
              "dn_out_g", "mla_cq_g", "mla_ckv_g", "mla_w_uq", "mla_w_ukv", "mla_qn_g", "mla_kn_g", "router_w",
              "router_b", "exp_w1", "exp_w2", "exp_b2"):
        sh[k] = f(inp[k])
    rb = f(inp["na_rel_bias"])
    g = rb[:, :, _NA_DR, _NA_DC]
    g = np.where(_NA_OK[None, None], g, np.float32(MASKNEG)).astype(np.float32)
    sh["na_bias"] = np.ascontiguousarray(g.transpose(0, 2, 1, 3, 4))
    cw = f(inp["dn_conv_w"])
    sh["dn_conv_l"] = np.ascontiguousarray(cw.reshape(DEPTH, 5, 6, 128).transpose(0, 3, 2, 1))
    sh["dn_a_log"] = f(inp["dn_a_log"]).reshape(DEPTH, 8)
    sh["dn_dt_bias"] = f(inp["dn_dt_bias"]).reshape(DEPTH, 8)
    b1 = f(inp["exp_b1"])
    sh["exp_b1_l"] = np.ascontiguousarray(b1.reshape(DEPTH, NE, 16, 128).transpose(0, 3, 1, 2))
    cg, sg = _rope_tables(64)
    cm, sm = _rope_tables(32)
    sh["cos_g"], sh["sin_g"], sh["cos_m"], sh["sin_m"] = cg, sg, cm, sm
    sh["dn_consts"] = _dn_consts()
    s65 = np.zeros((65, 64), np.float32); s65[64, :] = 1.0
    sh["sel65"] = s65
    bk = np.zeros((128, 128), np.float32); bk[:64, :64] = 1.0; bk[64:, 64:] = 1.0
    sh["blk64"] = bk
    sh["ident"] = np.eye(128, dtype=np.float32)
    sh["cctx_l"] = np.ascontiguousarray(f(inp["c_ctx"]).reshape(8, 128).T)
    return sh


def _core_inputs(inp, sh, b):
    m = dict(sh)
    m["x"] = np.ascontiguousarray(np.asarray(inp["x"][b], dtype=np.float32))
    m["ctx"] = np.ascontiguousarray(np.asarray(inp["ctx"][b], dtype=np.float32))
    m["c_l"] = np.ascontiguousarray(np.asarray(inp["c"][b], dtype=np.float32).reshape(8, 128).T)
    return m


_NC_CACHE = {}


def kernel(**inputs):
    if "nc" not in _NC_CACHE:
        _NC_CACHE["nc"] = Builder().build()
    nc = _NC_CACHE["nc"]
    sh = _shared_inputs(inputs)
    B = inputs["x"].shape[0]
    in_maps = [_core_inputs(inputs, sh, b) for b in range(B)]
    res = run_bass_kernel_spmd(nc, in_maps, core_ids=list(range(B)))
    return np.stack([np.asarray(r["out"], dtype=np.float32) for r in res.results], axis=0)
```

```python
import contextlib
import numpy as np
import concourse.bass as bass
import concourse.mybir as mybir
from concourse.bass_utils import run_bass_kernel_spmd

F32 = mybir.dt.float32
BF16 = mybir.dt.bfloat16
ALU = mybir.AluOpType
AF = mybir.ActivationFunctionType
AX = mybir.AxisListType

D = 1024
NLAT = 4096
NCTX = 256
T = NLAT + NCTX
NT = T // 128
NTL = NLAT // 128
DEPTH = 2
INW = 2736
NE = 32
EPS = 1e-6
MASKNEG = -30000.0


def _key(x):
    return x if isinstance(x, str) else x.name


class Sched:
    NDMA = 12

    def __init__(self, nc, stack):
        self.nc = nc
        self.E = {"pe": nc.tensor, "dve": nc.vector, "act": nc.scalar, "pool": nc.gpsimd, "sp": nc.sync}
        self.sem, self.cnt = {}, {}
        for e in ("pe", "dve", "act", "pool"):
            self.sem[e] = stack.enter_context(nc.semaphore("s_" + e))
            self.cnt[e] = 0
        self.dsem, self.dval, self.dnext = {}, {}, {}
        for q in ("sp", "act", "pool"):
            self.dsem[q] = [stack.enter_context(nc.semaphore(f"d_{q}{i}")) for i in range(self.NDMA)]
            self.dval[q] = [0] * self.NDMA
            self.dnext[q] = 0
        self.waited, self.semobj, self.W, self.R = {}, {}, {}, {}
        self.psum_keys = set()
        self.ninst = 0
        self.out_tokens = []

    def _tok(self, sem, val):
        self.semobj[sem.name] = sem
        return (sem.name, val)

    def _wait(self, eng, toks):
        need = {}
        for t in toks:
            if t is None:
                continue
            n, v = t
            if v > need.get(n, 0):
                need[n] = v
        for n, v in need.items():
            k = (eng, n)
            if self.waited.get(k, 0) >= v:
                continue
            if eng == "pe" and n == "s_pe":
                continue
            self.E[eng].wait_ge(self.semobj[n], v)
            self.waited[k] = v
            self.ninst += 1

    def _deps(self, reads, writes):
        toks = []
        for k in reads:
            toks.append(self.W.get(k))
            if k in self.psum_keys:
                toks.extend(self.R.get(k, ()))
        for k in writes:
            toks.append(self.W.get(k))
            toks.extend(self.R.get(k, ()))
        return toks

    def _commit(self, tok, reads, writes):
        for k in reads:
            lst = self.R.setdefault(k, [])
            lst.append(tok)
            if len(lst) > 16:
                best = {}
                for n, v in lst:
                    if v > best.get(n, 0):
                        best[n] = v
                self.R[k] = list(best.items())
        for k in writes:
            self.W[k] = tok
            self.R[k] = []

    def op(self, eng, fn, reads=(), writes=()):
        reads = [_key(r) for r in reads]
        writes = [_key(w) for w in writes]
        self._wait(eng, self._deps(reads, writes))
        ins = fn()
        self.cnt[eng] += 1
        ins.then_inc(self.sem[eng], 1)
        tok = self._tok(self.sem[eng], self.cnt[eng])
        self._commit(tok, reads, writes)
        self.ninst += 1
        return tok

    def dma(self, q, out, in_, reads=(), writes=(), is_output=False):
        reads = [_key(r) for r in reads]
        writes = [_key(w) for w in writes]
        i = self.dnext[q]
        self.dnext[q] = (i + 1) % self.NDMA
        sem = self.dsem[q][i]
        prev = self._tok(sem, self.dval[q][i]) if self.dval[q][i] else None
        self._wait(q, self._deps(reads, writes) + [prev])
        ins = self.E[q].dma_start(out=out, in_=in_)
        self.dval[q][i] += 16
        ins.then_inc(sem, 16)
        tok = self._tok(sem, self.dval[q][i])
        self._commit(tok, reads, writes)
        self.ninst += 1
        if is_output:
            self.out_tokens.append(tok)
        return tok

    def _all_tokens(self):
        toks = []
        for e in ("pe", "dve", "act", "pool"):
            if self.cnt[e]:
                toks.append(self._tok(self.sem[e], self.cnt[e]))
        for q in self.dsem:
            for i, s in enumerate(self.dsem[q]):
                if self.dval[q][i]:
                    toks.append(self._tok(s, self.dval[q][i]))
        return toks

    def barrier(self):
        toks = self._all_tokens()
        for e in ("pe", "dve", "act", "pool", "sp"):
            self._wait(e, toks)
        self.W, self.R = {}, {}

    def finish(self):
        self._wait("sp", self._all_tokens() + self.out_tokens)


def _rope_tables(rot_dim):
    t = np.arange(NLAT)
    row = (t // 64).astype(np.float32)
    col = (t % 64).astype(np.float32)
    nf = rot_dim // 4
    freqs = (np.float32(10000.0) ** (-np.arange(nf, dtype=np.float32) / np.float32(nf))).astype(np.float32)
    ang = np.concatenate([row[:, None] * freqs, col[:, None] * freqs], axis=-1).astype(np.float32)
    return np.cos(ang).astype(np.float32), np.sin(ang).astype(np.float32)


def _na_patterns():
    pats, plan, store = {}, [], []
    i = np.arange(128)
    for t in range(NTL):
        qr = 2 * t + i // 64
        qc = i % 64
        r0 = np.clip(qr - 4, 0, 56)
        c0 = np.clip(qc - 8, 0, 48)
        lst = []
        for u in range(NTL):
            kr = 2 * u + i // 64
            kc = i % 64
            ok = ((kr[:, None] >= r0[None, :]) & (kr[:, None] < r0[None, :] + 8)
                  & (kc[:, None] >= c0[None, :]) & (kc[:, None] < c0[None, :] + 16))
            if not ok.any():
                continue
            dr = np.clip(kr[:, None] - qr[None, :] + 7, 0, 14)
            dc = np.clip(kc[:, None] - qc[None, :], -15, 15) + 15
            key = (ok.tobytes(), (dr * ok).tobytes(), (dc * ok).tobytes())
            if key not in pats:
                pats[key] = len(store)
                store.append((dr, dc, ok))
            lst.append((u, pats[key]))
        plan.append(lst)
    dr = np.stack([s[0] for s in store])
    dc = np.stack([s[1] for s in store])
    ok = np.stack([s[2] for s in store])
    return plan, dr, dc, ok


_NA_PLAN, _NA_DR, _NA_DC, _NA_OK = _na_patterns()
NPAT = _NA_DR.shape[0]


class Builder:
    def __init__(self, depth=DEPTH, debug=False, stages=None, tile_list=None):
        self.tile_list = tile_list
        self.cut = 99
        self.depth = depth
        self.debug = debug
        self.stages = stages
        self.nc = bass.Bass("TRN2", target_bir_lowering=False)
        self.dram = {}

    def sbuf(self, name, shape, dt):
        self._uid = getattr(self, "_uid", 0) + 1
        return self.nc.sbuf_tensor(f"{name}_u{self._uid}", shape, dt)

    def psum(self, name, shape, dt):
        self._uid = getattr(self, "_uid", 0) + 1
        self.S.psum_keys.add(f"{name}_u{self._uid}")
        return self.nc.psum_tensor(f"{name}_u{self._uid}", shape, dt)

    def din(self, name, shape, dt=F32):
        self.dram[name] = self.nc.dram_tensor(name, list(shape), dt, kind="ExternalInput").ap()
        return self.dram[name]

    def dscr(self, name, shape, dt=F32):
        kind = "ExternalOutput" if self.debug else "Internal"
        self.dram[name] = self.nc.dram_tensor(name, list(shape), dt, kind=kind).ap()
        return self.dram[name]

    def declare(self):
        dp = self.depth
        self.din("x", [NLAT, D]); self.din("ctx", [NCTX, D])
        self.din("c_l", [128, 8]); self.din("cctx_l", [128, 8])
        self.din("ident", [128, 128])
        self.din("ada_w", [DEPTH, D, 6 * D]); self.din("ada_b", [DEPTH, 6 * D])
        self.din("norm1_g", [DEPTH, D]); self.din("norm2_g", [DEPTH, D])
        self.din("w_in", [DEPTH, D, INW]); self.din("w_out", [DEPTH, D, D])
        self.din("na_qn_g", [DEPTH, 64]); self.din("na_kn_g", [DEPTH, 64])
        self.din("na_bias", [DEPTH, NPAT, 4, 128, 128])
        self.din("gqa_qn_g", [DEPTH, 64]); self.din("gqa_kn_g", [DEPTH, 64])
        self.din("dn_conv_l", [DEPTH, 128, 6, 5])
        self.din("dn_a_log", [DEPTH, 8]); self.din("dn_dt_bias", [DEPTH, 8]); self.din("dn_out_g", [DEPTH, 64])
        self.din("mla_cq_g", [DEPTH, 256]); self.din("mla_ckv_g", [DEPTH, 128])
        self.din("mla_w_uq", [DEPTH, 256, 384]); self.din("mla_w_ukv", [DEPTH, 128, 512])
        self.din("mla_qn_g", [DEPTH, 96]); self.din("mla_kn_g", [DEPTH, 96])
        self.din("router_w", [DEPTH, D, NE]); self.din("router_b", [DEPTH, NE])
        if self.want("MOE"):
            self.din("exp_w1", [DEPTH, NE, D, 2 * D]); self.din("exp_b1_l", [DEPTH, 128, NE, 16])
            self.din("exp_w2", [DEPTH, NE, D, D]); self.din("exp_b2", [DEPTH, NE, D])
        self.din("cos_g", [NLAT, 32]); self.din("sin_g", [NLAT, 32])
        self.din("cos_m", [NLAT, 16]); self.din("sin_m", [NLAT, 16])
        self.din("dn_consts", [64, 7, 8, 64])
        self.din("sel65", [65, 64]); self.din("blk64", [128, 128])
        self.out = self.nc.dram_tensor("out", [NLAT, D], F32, kind="ExternalOutput").ap()
        self.dscr("modv", [2, 128, 6 * D])
        self.dscr("qkT_na", [8, 64, T], BF16); self.dscr("V_na", [T, 4 * 65], BF16)
        self.dscr("qkT_gq", [6, 64, T], BF16); self.dscr("V_gq", [T, 2 * 65], BF16)
        self.dscr("qT_ml", [4, 96, T], BF16); self.dscr("kT_ml", [4, 96, T], BF16); self.dscr("V_ml", [T, 4 * 65], BF16)
        self.dscr("dnraw", [768, T]); self.dscr("dn_gate", [T, 256]); self.dscr("dn_ba", [T, 16])
        self.dscr("dnT2", [12, 64, T]); self.dscr("o_dn", [2, T, 256])
        self.dscr("yT", [D, T], BF16)
        self.dscr("x1", [T, D]); self.dscr("h2T", [8, 128, T], BF16); self.dscr("combT", [NE, T])
        self.dscr("xs", [T, D])

    def build(self):
        nc = self.nc
        self.declare()
        with contextlib.ExitStack() as st:
            self.S = Sched(nc, st)
            self.idf = st.enter_context(self.sbuf("idf", [128, 128], F32))
            self.idb = st.enter_context(self.sbuf("idb", [128, 128], BF16))
            self.scB = st.enter_context(self.sbuf("scB", [128, 2, 8, 128], BF16))
            S = self.S
            S.dma("sp", self.idf[:], self.dram["ident"][:, :], writes=[self.idf])
            S.op("dve", lambda: nc.vector.tensor_copy(self.idb[:], self.idf[:]), [self.idf], [self.idb])
            self.stage_silu_c()
            for l in range(self.depth):
                last = (l == DEPTH - 1)
                xin = (self.dram["x"], self.dram["ctx"]) if l == 0 else (self.dram["xs"][0:NLAT], self.dram["xs"][NLAT:T])
                if self.want("S0"): self.stage_adaln(l)
                if self.want("SA"): self.stage_a(l, xin)
                if self.want("GQ"): self.stage_attn(l, "gq", with_ctx=not last)
                if self.want("ML"): self.stage_attn(l, "ml", with_ctx=not last)
                if self.want("NA"): self.stage_na(l, with_ctx=not last)
                if self.want("DN"): self.stage_dn(l, with_ctx=not last)
                if self.want("SO"): self.stage_out(l, xin, with_ctx=not last)
                if self.want("MOE"): self.stage_moe(l, with_ctx=not last)
            S.barrier()
            S.finish()
        return nc

    def want(self, s):
        return self.stages is None or s in self.stages

    def stage_silu_c(self):
        nc, S = self.nc, self.S
        with contextlib.ExitStack() as st:
            cl = st.enter_context(self.sbuf("cl", [128, 2, 8], F32))
            S.dma("sp", cl[:, 0, :], self.dram["c_l"][:, :], writes=[cl])
            S.dma("sp", cl[:, 1, :], self.dram["cctx_l"][:, :], writes=[cl])
            S.op("act", lambda: nc.scalar.activation(cl[:], cl[:], AF.Silu), [cl], [cl])
            for v in range(2):
                for k in range(8):
                    S.op("dve", lambda: nc.vector.tensor_copy(self.scB[:, v, k, :], cl[:, v, k:k + 1].to_broadcast([128, 128])), [cl], [self.scB])
            S.barrier()

    def stage_adaln(self, l):
        nc, S = self.nc, self.S
        with contextlib.ExitStack() as st:
            wa = [st.enter_context(self.sbuf(f"wa{i}", [128, 8, 512], BF16)) for i in range(2)]
            bb = [st.enter_context(self.sbuf(f"bb{i}", [128, 512], F32)) for i in range(2)]
            mo = [st.enter_context(self.sbuf(f"mo{i}", [128, 512], F32)) for i in range(2)]
            pm = [st.enter_context(self.psum(f"pm{i}", [128, 512], F32)) for i in range(2)]
            aw = self.dram["ada_w"][l].rearrange("(k p) n -> p k n", p=128)
            for n in range(12):
                w_, b_ = wa[n % 2], bb[n % 2]
                S.dma("pool", w_[:], aw[:, :, n * 512:(n + 1) * 512], writes=[w_])
                S.dma("sp", b_[:], self.dram["ada_b"][l, n * 512:(n + 1) * 512].partition_broadcast(128), writes=[b_])
                for v in range(2):
                    p_, m_ = pm[v], mo[v]
                    for k in range(8):
                        S.op("pe", lambda: nc.tensor.matmul(p_[:], self.scB[:, v, k, :], w_[:, k, :], start=(k == 0), stop=(k == 7)), [self.scB, w_], [p_])
                    S.op("dve", lambda: nc.vector.tensor_tensor(m_[:], p_[:], b_[:], ALU.add), [p_, b_], [m_])
                    S.dma("sp", self.dram["modv"][v, :, n * 512:(n + 1) * 512], m_[:], reads=[m_])
            S.barrier()

    def load_bcast(self, st, name, src_row_ap, n, q="sp"):
        t = st.enter_context(self.sbuf(name, [128, n], F32))
        self.S.dma(q, t[:], src_row_ap.partition_broadcast(128), writes=[t])
        return t

    def headnorm_k(self, src, src_keys, H, d, gain, out, sq, ss, tmp, _a=None, _b=None, _c=None, mean=True):
        nc, S = self.nc, self.S
        P = src.shape[0]
        S.op("act", lambda: nc.scalar.activation(sq, src, AF.Square), src_keys, [sq])
        S.op("dve", lambda: nc.vector.tensor_reduce(ss, sq, AX.X, ALU.add), [sq], [ss])
        S.op("act", lambda: nc.scalar.activation(ss, ss, AF.Sqrt, scale=(1.0 / d if mean else 1.0), bias=EPS), [ss], [ss])
        S.op("dve", lambda: nc.vector.reciprocal(ss, ss), [ss], [ss])
        bc = ss.unsqueeze(2).to_broadcast([P, H, d])
        if gain is None:
            S.op("dve", lambda: nc.vector.tensor_tensor(out, src, bc, ALU.mult), list(src_keys) + [ss], [out])
        else:
            S.op("dve", lambda: nc.vector.tensor_tensor(tmp, src, bc, ALU.mult), list(src_keys) + [ss], [tmp])
            S.op("pool", lambda: nc.gpsimd.tensor_tensor(out, tmp, gain, ALU.mult), [tmp, gain], [out])

    def rope(self, src, out, H, npair, cs, sn, ta, tb, src_k=None, out_k=None, cs_k=None, ta_k=None, tb_k=None):
        nc, S = self.nc, self.S
        x1, x2 = src[:, :, :, 0], src[:, :, :, 1]
        csb = cs.unsqueeze(1).to_broadcast([128, H, npair])
        snb = sn.unsqueeze(1).to_broadcast([128, H, npair])
        S.op("dve", lambda: nc.vector.tensor_tensor(ta, x1, csb, ALU.mult), [src_k, cs_k], [ta_k])
        S.op("pool", lambda: nc.gpsimd.tensor_tensor(tb, x2, snb, ALU.mult), [src_k, cs_k], [tb_k])
        S.op("dve", lambda: nc.vector.tensor_tensor(out[:, :, :, 0], ta, tb, ALU.subtract), [ta_k, tb_k], [out_k])
        S.op("dve", lambda: nc.vector.tensor_tensor(ta, x1, snb, ALU.mult), [src_k, cs_k], [ta_k])
        S.op("pool", lambda: nc.gpsimd.tensor_tensor(tb, x2, csb, ALU.mult), [src_k, cs_k], [tb_k])
        S.op("dve", lambda: nc.vector.tensor_tensor(out[:, :, :, 1], ta, tb, ALU.add), [ta_k, tb_k], [out_k])

    def stage_a(self, l, xin):
        nc, S, dr = self.nc, self.S, self.dram
        with contextlib.ExitStack() as st:
            sb = lambda name, shape, dt=F32: st.enter_context(self.sbuf(name, shape, dt))
            ps = lambda name, shape, dt=F32: st.enter_context(self.psum(name, shape, dt))
            win = sb("win", [128, 8, INW], BF16)
            wsrc = dr["w_in"][l].rearrange("(k p) n -> p k n", p=128)
            for k in range(0, 8, 2):
                S.dma("pool", win[:, k:k + 2, :], wsrc[:, k:k + 2, :], writes=[f"win{k}"])
            wink = [f"win{k - k % 2}" for k in range(8)]
            wuq = sb("wuq", [128, 2, 384], BF16)
            S.dma("pool", wuq[:], dr["mla_w_uq"][l].rearrange("(k p) n -> p k n", p=128), writes=[wuq])
            wukv = sb("wukv", [128, 512], BF16)
            S.dma("pool", wukv[:], dr["mla_w_ukv"][l], writes=[wukv])
            G, SH = [], []
            g1 = self.load_bcast(st, "g1", dr["norm1_g"][l], D)
            for v in range(2):
                Gv = sb(f"G{v}", [128, D]); Sv = sb(f"SH{v}", [128, D])
                S.dma("sp", Sv[:], dr["modv"][v, :, 0:D], writes=[Sv])
                S.dma("sp", Gv[:], dr["modv"][v, :, D:2 * D], writes=[Gv])
                S.op("dve", lambda: nc.vector.scalar_tensor_tensor(Gv[:], Gv[:], 1.0, g1[:], ALU.add, ALU.mult), [Gv, g1], [Gv])
                G.append(Gv); SH.append(Sv)
            gna = sb("gna", [128, 8, 64]); ggq = sb("ggq", [128, 6, 64]); gmq = sb("gmq", [128, 4, 96]); gmk = sb("gmk", [128, 4, 96])
            gt = self.load_bcast(st, "gt_naq", dr["na_qn_g"][l], 64)
            S.op("dve", lambda: nc.vector.tensor_scalar(gna[:, 0:4, :], gt[:].unsqueeze(1).to_broadcast([128, 4, 64]), 0.125, None, ALU.mult), [gt], [gna])
            gt = self.load_bcast(st, "gt_nak", dr["na_kn_g"][l], 64)
            S.op("dve", lambda: nc.vector.tensor_copy(gna[:, 4:8, :], gt[:].unsqueeze(1).to_broadcast([128, 4, 64])), [gt], [gna])
            gt = self.load_bcast(st, "gt_gqq", dr["gqa_qn_g"][l], 64)
            S.op("dve", lambda: nc.vector.tensor_scalar(ggq[:, 0:4, :], gt[:].unsqueeze(1).to_broadcast([128, 4, 64]), 0.125, None, ALU.mult), [gt], [ggq])
            gt = self.load_bcast(st, "gt_gqk", dr["gqa_kn_g"][l], 64)
            S.op("dve", lambda: nc.vector.tensor_copy(ggq[:, 4:6, :], gt[:].unsqueeze(1).to_broadcast([128, 2, 64])), [gt], [ggq])
            gt = self.load_bcast(st, "gt_mq", dr["mla_qn_g"][l], 96)
            S.op("dve", lambda: nc.vector.tensor_scalar(gmq[:], gt[:].unsqueeze(1).to_broadcast([128, 4, 96]), float(96 ** -0.5), None, ALU.mult), [gt], [gmq])
            gt = self.load_bcast(st, "gt_mk", dr["mla_kn_g"][l], 96)
            S.op("dve", lambda: nc.vector.tensor_copy(gmk[:], gt[:].unsqueeze(1).to_broadcast([128, 4, 96])), [gt], [gmk])
            gcq = self.load_bcast(st, "gcq", dr["mla_cq_g"][l], 256)
            gckv = self.load_bcast(st, "gckv", dr["mla_ckv_g"][l], 128)
            dbl = lambda name, shape, dt=F32: [sb(f"{name}_{i}", shape, dt) for i in range(2)]
            xt = dbl("xt", [128, D]); junks = dbl("junk", [128, D]); ssxs = dbl("ssx", [128, 1])
            hfs = dbl("hf", [128, D]); hbs = dbl("hb", [128, D], BF16); hTs = dbl("hT", [128, 8, 128], BF16)
            psbs = dbl("Tpsb", [128, INW]); dnTs = dbl("dnT", [128, 6, 128])
            sqs = dbl("sq", [128, 512]); sss = dbl("ss", [128, 8]); tmpns = dbl("tmpn", [128, 512])
            nbs = dbl("nb", [128, 8, 64], BF16); qkSs = dbl("qkS", [128, 8, 128], BF16)
            vaugs = dbl("vaug", [128, 4, 65], BF16); vaug2s = dbl("vaug2", [128, 2, 65], BF16); vaug3s = dbl("vaug3", [128, 4, 65], BF16)
            for va in vaugs + vaug2s + vaug3s:
                S.op("pool", lambda: nc.gpsimd.memset(va[:], 1.0), [], [va])
            gqfs = dbl("gqf", [128, 6, 64]); gqrs = dbl("gqr", [128, 6, 64], BF16)
            tas = dbl("ta", [128, 6 * 32]); tbs = dbl("tb", [128, 6 * 32]); rcs = dbl("rc", [128, 4, 32])
            cqns = dbl("cqn", [128, 256], BF16); cTs = dbl("cT", [128, 3, 128], BF16)
            mqs = dbl("mq", [128, 4, 96]); mqbs = dbl("mqb", [128, 4, 96], BF16)
            kfs = dbl("kf", [128, 4, 96]); mks = dbl("mk", [128, 4, 96]); mkbs = dbl("mkb", [128, 4, 96], BF16)
            pT = ps("pT", [128, 8, 128], BF16)
            pP = [ps(f"pP{i}", [128, 512]) for i in range(2)]
            pF = [ps(f"pF{i}", [128, 4, 128]) for i in range(2)]
            pQ = ps("pQ", [128, 8, 128], BF16)
            pM = ps("pM", [128, 512])
            chunks = [(0, 512), (512, 1024), (1024, 1280), (2048, 2560), (2560, 2736)]
            def body(t):
                par = t % 2
                junk, ssx, hf, hb, hT, psb, dnT = junks[par], ssxs[par], hfs[par], hbs[par], hTs[par], psbs[par], dnTs[par]
                sq, ss, tmpn, nb, qkS, vaug, vaug2, vaug3 = sqs[par], sss[par], tmpns[par], nbs[par], qkSs[par], vaugs[par], vaug2s[par], vaug3s[par]
                gqf, gqr, ta, tb, rc, cqn, cT = gqfs[par], gqrs[par], tas[par], tbs[par], rcs[par], cqns[par], cTs[par]
                mq, mqb, kf, mk, mkb = mqs[par], mqbs[par], kfs[par], mks[par], mkbs[par]
                rcg, rcm = "rcg%d" % par, "rcm%d" % par
                pk = lambda i: 'kpsb%d_%d' % (i, par)
                isc = t >= NTL
                v = 1 if isc else 0
                src = xin[1][(t - NTL) * 128:(t - NTL + 1) * 128, :] if isc else xin[0][t * 128:(t + 1) * 128, :]
                x_ = xt[t % 2]
                S.dma("sp", x_[:], src, writes=[x_])
                if not isc:
                    S.dma("act", rc[:, 0, :], dr["cos_g"][t * 128:(t + 1) * 128, :], writes=[rcg])
                    S.dma("act", rc[:, 1, :], dr["sin_g"][t * 128:(t + 1) * 128, :], writes=[rcg])
                    S.dma("act", rc[:, 2, 0:16], dr["cos_m"][t * 128:(t + 1) * 128, :], writes=[rcm])
                    S.dma("act", rc[:, 3, 0:16], dr["sin_m"][t * 128:(t + 1) * 128, :], writes=[rcm])
                S.op("act", lambda: nc.scalar.activation(junk[:], x_[:], AF.Square, accum_out=ssx[:]), [x_], [junk, ssx])
                S.op("act", lambda: nc.scalar.activation(ssx[:], ssx[:], AF.Sqrt, scale=1.0 / D, bias=EPS), [ssx], [ssx])
                S.op("dve", lambda: nc.vector.reciprocal(ssx[:], ssx[:]), [ssx], [ssx])
                S.op("dve", lambda: nc.vector.scalar_tensor_tensor(hf[:], x_[:], ssx[:, 0:1], G[v][:], ALU.mult, ALU.mult), [x_, ssx, G[v]], [hf])
                S.op("pool", lambda: nc.gpsimd.tensor_tensor(hb[:], hf[:], SH[v][:], ALU.add), [hf, SH[v]], [hb])
                yield
                for k in range(8):
                    S.op("pe", lambda: nc.tensor.transpose(pT[:, k, :], hb[:, k * 128:(k + 1) * 128], self.idb[:]), [hb, self.idb], [pT])
                S.op("act", lambda: nc.scalar.copy(hT[:], pT[:]), [pT], [hT])
                yield
                for ci, (a, b) in enumerate(chunks):
                    p_ = pP[ci % 2]
                    for k in range(8):
                        S.op("pe", lambda: nc.tensor.matmul(p_[:, 0:b - a], hT[:, k, :], win[:, k, a:b], start=(k == 0), stop=(k == 7)), [hT, wink[k]], [p_])
                    if ci % 2 == 0:
                        S.op("dve", lambda: nc.vector.tensor_copy(psb[:, a:b], p_[:, 0:b - a]), [p_], [pk(ci)])
                    else:
                        S.op("act", lambda: nc.scalar.copy(psb[:, a:b], p_[:, 0:b - a]), [p_], [pk(ci)])
                    yield
                for c in range(6):
                    pf = pF[c // 4]
                    for k in range(8):
                        S.op("pe", lambda: nc.tensor.matmul(pf[:, c % 4, :], win[:, k, 1280 + c * 128:1280 + (c + 1) * 128], hT[:, k, :], start=(k == 0), stop=(k == 7)), [hT, wink[k]], [pf])
                S.op("dve", lambda: nc.vector.tensor_copy(dnT[:, 0:4, :], pF[0][:]), [pF[0]], [dnT])
                S.op("act", lambda: nc.scalar.copy(dnT[:, 4:6, :], pF[1][:, 0:2, :]), [pF[1]], [dnT])
                yield
                S.dma("sp", dr["dnraw"].rearrange("(c p) n -> p c n", p=128)[:, :, t * 128:(t + 1) * 128], dnT[:], reads=[dnT])
                S.dma("sp", dr["dn_gate"][t * 128:(t + 1) * 128, :], psb[:, 2048:2304], reads=[pk(3)])
                S.dma("sp", dr["dn_ba"][t * 128:(t + 1) * 128, :], psb[:, 2304:2320], reads=[pk(3)])
                yield
                v3 = lambda ap, H: ap.rearrange("p (h d) -> p h d", h=H)
                self.headnorm_k(v3(psb[:, 0:512], 8), [pk(0)], 8, 64, gna[:], nb[:], v3(sq[:, 0:512], 8), ss[:, 0:8], v3(tmpn[:, 0:512], 8), [sq, ss, tmpn], gna, nb)
                yield
                for h in range(8):
                    S.op("pe", lambda: nc.tensor.transpose(pQ[0:64, h, :], nb[:, h, :], self.idb[:]), [nb, self.idb], [pQ])
                S.op("act", lambda: nc.scalar.copy(qkS[0:64, :, :], pQ[0:64, :, :]), [pQ], [qkS])
                yield
                S.dma("sp", dr["qkT_na"].rearrange("h d n -> d h n")[:, :, t * 128:(t + 1) * 128], qkS[0:64, :, :], reads=[qkS])
                S.op("pool", lambda: nc.gpsimd.tensor_copy(vaug[:, :, 0:64], v3(psb[:, 512:768], 4)), [pk(1)], [vaug])
                S.dma("sp", dr["V_na"][t * 128:(t + 1) * 128, :], vaug[:].rearrange("p h e -> p (h e)"), reads=[vaug])
                yield
                self.headnorm_k(v3(psb[:, 768:1152], 6), [pk(1), pk(2)], 6, 64, ggq[:], gqf[:], v3(sq[:, 0:384], 6), ss[:, 0:6], v3(tmpn[:, 0:384], 6), [sq, ss, tmpn], ggq, gqf)
                yield
                if isc:
                    S.op("dve", lambda: nc.vector.tensor_copy(gqr[:], gqf[:]), [gqf], [gqr])
                else:
                    v4 = lambda ap: ap.rearrange("p h (n two) -> p h n two", two=2)
                    self.rope(v4(gqf[:]), v4(gqr[:]), 6, 32, rc[:, 0, :], rc[:, 1, :], v3(ta[:], 6), v3(tb[:], 6), src_k=gqf, out_k=gqr, cs_k=rcg, ta_k=ta, tb_k=tb)
                for h in range(6):
                    S.op("pe", lambda: nc.tensor.transpose(pQ[0:64, h, :], gqr[:, h, :], self.idb[:]), [gqr, self.idb], [pQ])
                S.op("act", lambda: nc.scalar.copy(qkS[0:64, 0:6, :], pQ[0:64, 0:6, :]), [pQ], [qkS])
                yield
                S.dma("sp", dr["qkT_gq"].rearrange("h d n -> d h n")[:, :, t * 128:(t + 1) * 128], qkS[0:64, 0:6, :], reads=[qkS])
                S.op("pool", lambda: nc.gpsimd.tensor_copy(vaug2[:, :, 0:64], v3(psb[:, 1152:1280], 2)), [pk(2)], [vaug2])
                S.dma("sp", dr["V_gq"][t * 128:(t + 1) * 128, :], vaug2[:].rearrange("p h e -> p (h e)"), reads=[vaug2])
                yield
                v2 = lambda ap: ap.unsqueeze(1)
                self.headnorm_k(v2(psb[:, 2320:2576]), [pk(3), pk(4)], 1, 256, v2(gcq[:]), v2(cqn[:]), v2(sq[:, 0:256]), ss[:, 0:1], v2(tmpn[:, 0:256]), [sq, ss, tmpn], gcq, cqn)
                yield
                for k in range(2):
                    S.op("pe", lambda: nc.tensor.transpose(pT[:, k, :], cqn[:, k * 128:(k + 1) * 128], self.idb[:]), [cqn, self.idb], [pT])
                S.op("act", lambda: nc.scalar.copy(cT[:, 0:2, :], pT[:, 0:2, :]), [pT], [cT])
                yield
                for k in range(2):
                    S.op("pe", lambda: nc.tensor.matmul(pM[:, 0:384], cT[:, k, :], wuq[:, k, :], start=(k == 0), stop=(k == 1)), [cT, wuq], [pM])
                self.headnorm_k(v3(pM[:, 0:384], 4), [pM], 4, 96, gmq[:], mq[:], v3(sq[:, 0:384], 4), ss[:, 0:4], v3(tmpn[:, 0:384], 4), [sq, ss, tmpn], gmq, mq)
                yield
                S.op("pool", lambda: nc.gpsimd.tensor_copy(mqb[:], mq[:]), [mq], [mqb])
                if not isc:
                    v4m = lambda ap: ap[:, :, 64:96].rearrange("p h (n two) -> p h n two", two=2)
                    self.rope(v4m(mq[:]), v4m(mqb[:]), 4, 16, rc[:, 2, 0:16], rc[:, 3, 0:16], v3(ta[:, 0:64], 4), v3(tb[:, 0:64], 4), src_k=mq, out_k=mqb, cs_k=rcm, ta_k=ta, tb_k=tb)
                for h in range(4):
                    S.op("pe", lambda: nc.tensor.transpose(pQ[0:96, h, :], mqb[:, h, :], self.idb[:]), [mqb, self.idb], [pQ])
                S.op("act", lambda: nc.scalar.copy(qkS[0:96, 0:4, :], pQ[0:96, 0:4, :]), [pQ], [qkS])
                S.dma("sp", dr["qT_ml"].rearrange("h d n -> d h n")[:, :, t * 128:(t + 1) * 128], qkS[0:96, 0:4, :], reads=[qkS])
                yield
                self.headnorm_k(v2(psb[:, 2576:2704]), [pk(4)], 1, 128, v2(gckv[:]), v2(cqn[:, 0:128]), v2(sq[:, 0:128]), ss[:, 0:1], v2(tmpn[:, 0:128]), [sq, ss, tmpn], gckv, cqn)
                yield
                S.op("pe", lambda: nc.tensor.transpose(pT[:, 2, :], cqn[:, 0:128], self.idb[:]), [cqn, self.idb], [pT])
                S.op("act", lambda: nc.scalar.copy(cT[:, 2, :], pT[:, 2, :]), [pT], [cT])
                yield
                S.op("pe", lambda: nc.tensor.matmul(pM[:, :], cT[:, 2, :], wukv[:, :], start=True, stop=True), [cT, wukv], [pM])
                kv = pM[:, :].rearrange("p (h e) -> p h e", h=4)
                S.op("dve", lambda: nc.vector.tensor_copy(kf[:, :, 0:64], kv[:, :, 0:64]), [pM], [kf])
                S.op("dve", lambda: nc.vector.tensor_copy(kf[:, :, 64:96], psb[:, 2704:2736].unsqueeze(1).to_broadcast([128, 4, 32])), [pk(4)], [kf])
                S.op("dve", lambda: nc.vector.tensor_copy(vaug3[:, :, 0:64], kv[:, :, 64:128]), [pM], [vaug3])
                S.dma("sp", dr["V_ml"][t * 128:(t + 1) * 128, :], vaug3[:].rearrange("p h e -> p (h e)"), reads=[vaug3])
                yield
                self.headnorm_k(kf[:], [kf], 4, 96, gmk[:], mk[:], v3(sq[:, 0:384], 4), ss[:, 0:4], v3(tmpn[:, 0:384], 4), [sq, ss, tmpn], gmk, mk)
                yield
                S.op("pool", lambda: nc.gpsimd.tensor_copy(mkb[:], mk[:]), [mk], [mkb])
                if not isc:
                    self.rope(v4m(mk[:]), v4m(mkb[:]), 4, 16, rc[:, 2, 0:16], rc[:, 3, 0:16], v3(ta[:, 0:64], 4), v3(tb[:, 0:64], 4), src_k=mk, out_k=mkb, cs_k=rcm, ta_k=ta, tb_k=tb)
                for h in range(4):
                    S.op("pe", lambda: nc.tensor.transpose(pQ[0:96, h, :], mkb[:, h, :], self.idb[:]), [mkb, self.idb], [pQ])
                S.op("act", lambda: nc.scalar.copy(qkS[0:96, 0:4, :], pQ[0:96, 0:4, :]), [pQ], [qkS])
                S.dma("sp", dr["kT_ml"].rearrange("h d n -> d h n")[:, :, t * 128:(t + 1) * 128], qkS[0:96, 0:4, :], reads=[qkS])
            tiles = list(self.tile_list or range(NT))
            active, nxt = [], 0
            while nxt < len(tiles) or active:
                while len(active) < 2 and nxt < len(tiles):
                    active.append(body(tiles[nxt])); nxt += 1
                for g in list(active):
                    try:
                        next(g)
                    except StopIteration:
                        active.remove(g)
            S.barrier()

    def attn_core(self, st_tiles, kT, Vg, qT, dk, q0, qn, ktiles, yrow, bias_fn=None):
        nc, S, dr = self.nc, self.S, self.dram
        P, pS, pO, pD, Osb, rden, yb, sel65, sadd = st_tiles
        dkp = 128 if dk == 64 else dk
        self._blk = getattr(self, "_blk", 0) + 1
        po = pO[self._blk % 2]
        n = len(ktiles)
        for i in range(n + 1):
            if i < n:
                kt = ktiles[i]
                ps_ = pS[i % 2]
                S.op("pe", lambda: nc.tensor.matmul(ps_[:, 0:qn], kT[0:dkp, kt * 128:(kt + 1) * 128], qT[0:dkp, q0:q0 + qn], start=True, stop=True), [kT, qT], [ps_])
                p_ = P[i % 4]
                b = bias_fn(kt) if bias_fn else None
                if b is not None:
                    S.op("dve", lambda: nc.vector.tensor_tensor(sadd[:, 0:qn], ps_[:, 0:qn], b[0], ALU.add), [ps_, b[1]], [sadd])
                    S.op("act", lambda: nc.scalar.activation(p_[:, 0:qn], sadd[:, 0:qn], AF.Exp), [sadd], [p_])
                else:
                    S.op("act", lambda: nc.scalar.activation(p_[:, 0:qn], ps_[:, 0:qn], AF.Exp), [ps_], [p_])
            if i >= 1:
                j = i - 1
                kt = ktiles[j]
                p_ = P[j % 4]
                S.op("pe", lambda: nc.tensor.matmul(po[0:65, 0:qn], Vg[:, kt, :], p_[:, 0:qn], start=(j == 0), stop=(j == n - 1)), [Vg, p_], [po])
        S.op("act", lambda: nc.scalar.copy(Osb[:, 0:qn], po[0:65, 0:qn]), [po], [Osb])
        S.op("pe", lambda: nc.tensor.matmul(pD[0:64, 0:qn], sel65[:, :], Osb[:, 0:qn], start=True, stop=True), [sel65, Osb], [pD])
        S.op("dve", lambda: nc.vector.reciprocal(rden[:, 0:qn], pD[0:64, 0:qn]), [pD], [rden])
        S.op("pool", lambda: nc.gpsimd.tensor_tensor(yb[:, 0:qn], Osb[0:64, 0:qn], rden[:, 0:qn], ALU.mult), [Osb, rden], [yb])
        S.dma("sp", dr["yT"][yrow:yrow + 64, q0:q0 + qn], yb[:, 0:qn], reads=[yb])

    def attn_tiles(self, st):
        nc = self.nc
        sb = lambda name, shape, dt=F32: st.enter_context(self.sbuf(name, shape, dt))
        ps = lambda name, shape, dt=F32: st.enter_context(self.psum(name, shape, dt))
        P = [sb(f"P{i}", [128, 512], BF16) for i in range(4)]
        pS = [ps(f"pS{i}", [128, 512]) for i in range(2)]
        pO = [ps(f"pO{i}", [128, 512]) for i in range(2)]
        pD = ps("pD", [128, 512])
        Osb = sb("Osb", [65, 512]); rden = sb("rden", [64, 512]); yb = sb("yb", [64, 512], BF16)
        sel65 = sb("sel65_sb", [65, 64]); sadd = sb("sadd", [128, 512])
        self.S.dma("sp", sel65[:], self.dram["sel65"][:, :], writes=[sel65])
        return (P, pS, pO, pD, Osb, rden, yb, sel65, sadd)

    def stage_attn(self, l, kind, with_ctx):
        nc, S, dr = self.nc, self.S, self.dram
        if kind == "gq":
            qsrc, ksrc, V, dk, qpk, Hk, ybase = dr["qkT_gq"][0:4], dr["qkT_gq"][4:6], dr["V_gq"], 64, 2, 2, 256
        else:
            qsrc, ksrc, V, dk, qpk, Hk, ybase = dr["qT_ml"], dr["kT_ml"], dr["V_ml"], 96, 1, 4, 768
        with contextlib.ExitStack() as st:
            sb = lambda name, shape, dt=F32: st.enter_context(self.sbuf(name, shape, dt))
            tiles = self.attn_tiles(st)
            dkp = 128 if dk == 64 else dk
            kTs = [sb(f"kT{i}", [dkp, T], BF16) for i in range(2)]
            Vs = [sb(f"Vg{i}", [128, NT, 65], BF16) for i in range(2)]
            qTs = [sb(f"qT{i}", [dkp, T], BF16) for i in range(2)]
            if dkp != dk:
                for t_ in kTs + qTs:
                    S.op("pool", lambda: nc.gpsimd.memset(t_[dk:dkp, :], 0.0), [], [t_])
            blocks = [(b * 512, 512, list(range(NT))) for b in range(NLAT // 512)]
            if with_ctx:
                blocks.append((NLAT, NCTX, [NTL, NTL + 1]))
            hq = 0
            for g in range(Hk):
                kT, Vg = kTs[g % 2], Vs[g % 2]
                S.dma("sp", kT[0:dk, :], ksrc[g], writes=[kT])
                S.dma("act", Vg[:], V.rearrange("(n p) e -> p n e", p=128)[:, :, g * 65:(g + 1) * 65], writes=[Vg])
                for r in range(qpk):
                    h = g * qpk + r
                    qT = qTs[hq % 2]; hq += 1
                    S.dma("sp", qT[0:dk, :], qsrc[h], writes=[qT])
                    for (q0, qn, ktiles) in blocks:
                        self.attn_core(tiles, kT, Vg, qT, dk, q0, qn, ktiles, ybase + h * 64)
            S.barrier()

    def stage_na(self, l, with_ctx):
        nc, S, dr = self.nc, self.S, self.dram
        with contextlib.ExitStack() as st:
            sb = lambda name, shape, dt=F32: st.enter_context(self.sbuf(name, shape, dt))
            tiles = self.attn_tiles(st)
            kTn = sb("kTn", [128, 4, T], BF16); qTn = sb("qTn", [128, 4, T], BF16)
            for t_ in (kTn, qTn):
                S.op("pool", lambda: nc.gpsimd.memset(t_[64:128, :, :], 0.0), [], [t_])
            Vn = sb("Vn", [128, NT, 4 * 65], BF16)
            biasT = sb("biasT", [128, NPAT * 4, 128])
            S.dma("sp", kTn[0:64], dr["qkT_na"][4:8].rearrange("h d n -> d h n"), writes=[kTn])
            S.dma("sp", qTn[0:64], dr["qkT_na"][0:4].rearrange("h d n -> d h n"), writes=[qTn])
            S.dma("act", Vn[:], dr["V_na"].rearrange("(n p) e -> p n e", p=128), writes=[Vn])
            for pi in range(NPAT):
                S.dma("act", biasT[:, pi * 4:(pi + 1) * 4, :], dr["na_bias"][l, pi].rearrange("h j i -> j h i"), writes=[biasT])
            for t in range(NTL):
                plan = dict(_NA_PLAN[t])
                ktiles = sorted(plan.keys()) + [NTL, NTL + 1]
                for h in range(4):
                    bf = lambda kt: ((biasT[:, plan[kt] * 4 + h, :], biasT) if kt in plan else None)
                    self.attn_core(tiles, kTn[:, h, :], Vn[:, :, h * 65:(h + 1) * 65], qTn[:, h, :], 64, t * 128, 128, ktiles, h * 64, bias_fn=bf)
            if with_ctx:
                for h in range(4):
                    self.attn_core(tiles, kTn[:, h, :], Vn[:, :, h * 65:(h + 1) * 65], qTn[:, h, :], 64, NLAT, NCTX, [NTL, NTL + 1], h * 64)
            S.barrier()

    def stage_out(self, l, xin, with_ctx):
        nc, S, dr = self.nc, self.S, self.dram
        with contextlib.ExitStack() as st:
            sb = lambda name, shape, dt=F32: st.enter_context(self.sbuf(name, shape, dt))
            ps = lambda name, shape, dt=F32: st.enter_context(self.psum(name, shape, dt))
            wout = sb("wout", [128, 8, D], BF16)
            S.dma("pool", wout[:], dr["w_out"][l].rearrange("(k p) n -> p k n", p=128), writes=[wout])
            rw = sb("rw", [128, 8, NE])
            S.dma("sp", rw[:], dr["router_w"][l].rearrange("(k p) n -> p k n", p=128), writes=[rw])
            rb = self.load_bcast(st, "rb", dr["router_b"][l], NE)
            g2 = self.load_bcast(st, "g2", dr["norm2_g"][l], D)
            nv = 2 if with_ctx else 1
            GA, G2, SH2 = [], [], []
            for v in range(nv):
                ga = sb(f"GA{v}", [128, D]); Gv = sb(f"G2{v}", [128, D]); Sv = sb(f"SH2{v}", [128, D])
                S.dma("sp", ga[:], dr["modv"][v, :, 2 * D:3 * D], writes=[ga])
                S.dma("sp", Sv[:], dr["modv"][v, :, 3 * D:4 * D], writes=[Sv])
                S.dma("sp", Gv[:], dr["modv"][v, :, 4 * D:5 * D], writes=[Gv])
                S.op("dve", lambda: nc.vector.scalar_tensor_tensor(Gv[:], Gv[:], 1.0, g2[:], ALU.add, ALU.mult), [Gv, g2], [Gv])
                GA.append(ga); G2.append(Gv); SH2.append(Sv)
            yt = [sb(f"yt{i}", [128, 8, 128], BF16) for i in range(2)]
            xt = [sb(f"xo{i}", [128, D]) for i in range(2)]
            x1ts = [sb(f"x1t{i}", [128, D]) for i in range(2)]; tmps = [sb(f"tmpo{i}", [128, D]) for i in range(2)]; junks = [sb(f"junko{i}", [128, D]) for i in range(2)]
            ssx = sb("ssxo", [128, 1]); h2fs = [sb(f"h2f{i}", [128, D]) for i in range(2)]
            h2Tfs = [sb(f"h2Tf{i}", [128, 8, 128]) for i in range(2)]; h2Tbs = [sb(f"h2Tb{i}", [128, 8, 128], BF16) for i in range(2)]
            d2 = lambda name, shape: [sb(f"{name}_{i}", shape) for i in range(2)]
            lgs, m8s, nmxs, exs = d2("lg", [128, NE]), d2("m8", [128, 8]), d2("nmx", [128, 1]), d2("ex", [128, NE])
            msks, sms, cmbs, cmTs, ssxs = d2("msk", [128, NE]), d2("sm", [128, 1]), d2("cmb", [128, NE]), d2("cmT", [NE, 128]), d2("ssxo2", [128, 1])
            pP = [ps(f"pPo{i}", [128, 512]) for i in range(2)]
            pTt = [ps(f"pTt{i}", [128, 4, 128]) for i in range(2)]
            pL = ps("pL", [128, 512])
            ntile = NT if with_ctx else NTL
            def body(t):
                lg, m8, nmx, ex, msk, sm, cmb, cmT, ssx = lgs[t % 2], m8s[t % 2], nmxs[t % 2], exs[t % 2], msks[t % 2], sms[t % 2], cmbs[t % 2], cmTs[t % 2], ssxs[t % 2]
                isc = t >= NTL
                v = 1 if isc else 0
                src = xin[1][(t - NTL) * 128:(t - NTL + 1) * 128, :] if isc else xin[0][t * 128:(t + 1) * 128, :]
                x_, y_ = xt[t % 2], yt[t % 2]
                x1t, tmp, junk, h2f, h2Tf, h2Tb = x1ts[t % 2], tmps[t % 2], junks[t % 2], h2fs[t % 2], h2Tfs[t % 2], h2Tbs[t % 2]
                tk = t % 2
                S.dma("sp", x_[:], src, writes=[x_])
                S.dma("act", y_[:], dr["yT"].rearrange("(k p) n -> p k n", p=128)[:, :, t * 128:(t + 1) * 128], writes=[y_])
                for n in range(2):
                    for k in range(8):
                        S.op("pe", lambda: nc.tensor.matmul(pP[n][:], y_[:, k, :], wout[:, k, n * 512:(n + 1) * 512], start=(k == 0), stop=(k == 7)), [y_, wout], [pP[n]])
                    S.op("dve", lambda: nc.vector.tensor_tensor(tmp[:, n * 512:(n + 1) * 512], pP[n][:], GA[v][:, n * 512:(n + 1) * 512], ALU.mult), [pP[n], GA[v]], [f"tmpk{n}_{tk}"])
                    S.op("pool", lambda: nc.gpsimd.tensor_tensor(x1t[:, n * 512:(n + 1) * 512], tmp[:, n * 512:(n + 1) * 512], x_[:, n * 512:(n + 1) * 512], ALU.add), [f"tmpk{n}_{tk}", x_], [f"x1k{n}_{tk}"])
                    yield
                S.dma("sp", dr["x1"][t * 128:(t + 1) * 128, :], x1t[:], reads=[f"x1k0_{tk}", f"x1k1_{tk}"])
                S.op("act", lambda: nc.scalar.activation(junk[:], x1t[:], AF.Square, accum_out=ssx[:]), [f"x1k0_{tk}", f"x1k1_{tk}"], [junk, ssx])
                S.op("act", lambda: nc.scalar.activation(ssx[:], ssx[:], AF.Sqrt, scale=1.0 / D, bias=EPS), [ssx], [ssx])
                S.op("dve", lambda: nc.vector.reciprocal(ssx[:], ssx[:]), [ssx], [ssx])
                S.op("dve", lambda: nc.vector.scalar_tensor_tensor(junk[:], x1t[:], ssx[:, 0:1], G2[v][:], ALU.mult, ALU.mult), [f"x1k0_{tk}", f"x1k1_{tk}", ssx, G2[v]], [junk])
                S.op("pool", lambda: nc.gpsimd.tensor_tensor(h2f[:], junk[:], SH2[v][:], ALU.add), [junk, SH2[v]], [h2f])
                yield
                for k in range(8):
                    S.op("pe", lambda: nc.tensor.transpose(pTt[k // 4][:, k % 4, :], h2f[:, k * 128:(k + 1) * 128], self.idf[:]), [h2f, self.idf], [pTt[k // 4]])
                for hh in range(2):
                    S.op("act", lambda: nc.scalar.copy(h2Tf[:, hh * 4:(hh + 1) * 4, :], pTt[hh][:]), [pTt[hh]], [h2Tf])
                    S.op("dve", lambda: nc.vector.tensor_copy(h2Tb[:, hh * 4:(hh + 1) * 4, :], pTt[hh][:]), [pTt[hh]], [h2Tb])
                S.dma("sp", dr["h2T"].rearrange("k p n -> p k n")[:, :, t * 128:(t + 1) * 128], h2Tb[:], reads=[h2Tb])
                yield
                for k in range(8):
                    S.op("pe", lambda: nc.tensor.matmul(pL[:, 0:NE], h2Tf[:, k, :], rw[:, k, :], start=(k == 0), stop=(k == 7)), [h2Tf, rw], [pL])
                S.op("dve", lambda: nc.vector.tensor_tensor(lg[:], pL[:, 0:NE], rb[:], ALU.add), [pL, rb], [lg])
                yield
                S.op("dve", lambda: nc.vector.max(m8[:], lg[:]), [lg], [m8])
                S.op("dve", lambda: nc.vector.tensor_scalar(nmx[:], m8[:, 0:1], -1.0, None, ALU.mult), [m8], [nmx])
                S.op("act", lambda: nc.scalar.activation(ex[:], lg[:], AF.Exp, bias=nmx[:, 0:1], scale=1.0), [lg, nmx], [ex])
                S.op("dve", lambda: nc.vector.tensor_scalar(msk[:], lg[:], m8[:, 3:4], None, ALU.is_ge), [lg, m8], [msk])
                S.op("dve", lambda: nc.vector.tensor_tensor(ex[:], ex[:], msk[:], ALU.mult), [ex, msk], [ex])
                S.op("dve", lambda: nc.vector.tensor_reduce(sm[:], ex[:], AX.X, ALU.add), [ex], [sm])
                S.op("dve", lambda: nc.vector.reciprocal(sm[:], sm[:]), [sm], [sm])
                S.op("dve", lambda: nc.vector.tensor_scalar(cmb[:], ex[:], sm[:, 0:1], None, ALU.mult), [ex, sm], [cmb])
                yield
                S.op("pe", lambda: nc.tensor.transpose(pL[0:NE, 128:256], cmb[:], self.idf[:]), [cmb, self.idf], [pL])
                S.op("act", lambda: nc.scalar.copy(cmT[:], pL[0:NE, 128:256]), [pL], [cmT])
                S.dma("sp", dr["combT"][:, t * 128:(t + 1) * 128], cmT[:], reads=[cmT])
            active, nxt = [], 0
            while nxt < ntile or active:
                while len(active) < 2 and nxt < ntile:
                    active.append(body(nxt)); nxt += 1
                for g in list(active):
                    try:
                        next(g)
                    except StopIteration:
                        active.remove(g)
            S.barrier()

    def stage_moe(self, l, with_ctx):
        nc, S, dr = self.nc, self.S, self.dram
        ntile = NT if with_ctx else NTL
        sizes = [7, 7, 7, 7, 6] if with_ctx else [8, 8, 8, 8]
        last = (l == DEPTH - 1)
        with contextlib.ExitStack() as st:
            sb = lambda name, shape, dt=F32: st.enter_context(self.sbuf(name, shape, dt))
            ps = lambda name, shape, dt=F32: st.enter_context(self.psum(name, shape, dt))
            GMAX = 8 * 128
            h2g = sb("h2g", [128, 8, GMAX], BF16); acc = sb("acc", [128, 8, GMAX])
            W1 = [sb(f"W1_{i}", [128, 8, 2 * D], BF16) for i in range(2)]
            W2 = sb("W2", [128, 8, D], BF16)
            actT = [sb(f"actT{i}", [128, 8, 512], BF16) for i in range(2)]
            tr3 = [sb(f"tr3{i}", [128, 512]) for i in range(2)]
            tsg = [sb(f"tsg{i}", [128, 512]) for i in range(2)]
            tr1 = [sb(f"tr1{i}", [128, 512]) for i in range(2)]
            tr2 = [sb(f"tr2{i}", [128, 512]) for i in range(2)]
            tng = [sb(f"tng{i}", [128, 512]) for i in range(2)]
            b1m = sb("b1m", [128, NE, 16]); sgb = sb("sgb", [128, 1]); c14 = sb("c14", [128, 1])
            cB = [sb(f"cB{i}", [128, GMAX]) for i in range(2)]
            b1 = sb("b1", [128, NE, 16])
            b2b = sb("b2b", [NE, D], BF16); cTb = sb("cTb", [NE, GMAX], BF16)
            S.dma("sp", b1[:], dr["exp_b1_l"][l], writes=[b1])
            S.op("dve", lambda: nc.vector.tensor_scalar(b1m[:], b1[:], -1.0, 7.0, ALU.mult, ALU.add), [b1], [b1m])
            S.op("dve", lambda: nc.vector.memset(sgb[:], 1.702 * 7.0), [], [sgb])
            S.op("dve", lambda: nc.vector.memset(c14[:], 14.0), [], [c14])
            S.dma("pool", b2b[:], dr["exp_b2"][l], writes=[b2b])
            nv = 2 if with_ctx else 1
            G6 = []
            for v in range(nv):
                g6 = sb(f"G6{v}", [128, D])
                S.dma("sp", g6[:], dr["modv"][v, :, 5 * D:6 * D], writes=[g6])
                G6.append(g6)
            x1t = sb("x1m", [128, D]); tmpf = sb("tmpf", [128, D]); x2t = sb("x2m", [128, D])
            pg = [ps(f"pg{i}", [128, 512]) for i in range(2)]
            pl = [ps(f"pl{i}", [128, 512]) for i in range(2)]
            po = [ps(f"po{i}", [128, 512]) for i in range(2)]
            pF = [ps(f"pFm{i}", [128, 4, 128]) for i in range(2)]
            t0 = 0
            cnt = 0
            for gsz in sizes:
                g0, gn = t0 * 128, gsz * 128
                S.dma("sp", h2g[:, :, 0:gn], dr["h2T"].rearrange("k p n -> p k n")[:, :, g0:g0 + gn], writes=[h2g])
                S.dma("pool", cTb[:, 0:gn], dr["combT"][:, g0:g0 + gn], writes=[cTb])
                blocks = [(b0, min(512, gn - b0)) for b0 in range(0, gn, 512)]
                for e in range(NE):
                    w1 = W1[e % 2]
                    w1k = [f"{w1.name}h{k // 4}" for k in range(8)]
                    if not getattr(self, "moe_no_dma", False):
                        for hk in range(2):
                            S.dma("pool", w1[:, hk * 4:(hk + 1) * 4, :], dr["exp_w1"][l, e].rearrange("(k p) n -> p k n", p=128)[:, hk * 4:(hk + 1) * 4, :], writes=[f"{w1.name}h{hk}"])
                        S.dma("pool", W2[:], dr["exp_w2"][l, e].rearrange("(k p) n -> p k n", p=128), writes=[W2])
                    cb = cB[e % 2]
                    S.dma("sp", cb[:, 0:gn], dr["combT"][e, g0:g0 + gn].partition_broadcast(128), writes=[cb])
                    S.op("dve", lambda: nc.vector.tensor_scalar(cb[:, 0:gn], cb[:, 0:gn], -1.0, None, ALU.mult), [cb], [cb])
                    for (b0, bn) in blocks:
                        aT = actT[cnt % 2]; cnt += 1
                        for j in range(8):
                            i2 = j % 2
                            for k in range(8):
                                S.op("pe", lambda: nc.tensor.matmul(pg[i2][:, 0:bn], w1[:, k, j * 128:(j + 1) * 128], h2g[:, k, b0:b0 + bn], start=(k == 0), stop=(k == 7)), [w1k[k], h2g], [pg[i2]])
                            for k in range(8):
                                S.op("pe", lambda: nc.tensor.matmul(pl[i2][:, 0:bn], w1[:, k, D + j * 128:D + (j + 1) * 128], h2g[:, k, b0:b0 + bn], start=(k == 0), stop=(k == 7)), [w1k[k], h2g], [pl[i2]])
                            if getattr(self, "moe_pe_only", False): continue
                            r3, sg_, r1, r2, ng = tr3[i2], tsg[i2], tr1[i2], tr2[i2], tng[i2]
                            S.op("act", lambda: nc.scalar.activation(r3[:, 0:bn], pg[i2][:, 0:bn], AF.Relu, bias=b1m[:, e, j:j + 1], scale=-1.0), [pg[i2], b1m], [r3])
                            S.op("act", lambda: nc.scalar.activation(sg_[:, 0:bn], r3[:, 0:bn], AF.Sigmoid, bias=sgb[:, 0:1], scale=-1.702), [r3, sgb], [sg_])
                            S.op("act", lambda: nc.scalar.activation(r1[:, 0:bn], pl[i2][:, 0:bn], AF.Relu, bias=b1m[:, e, 8 + j:9 + j], scale=-1.0), [pl[i2], b1m], [r1])
                            S.op("act", lambda: nc.scalar.activation(r2[:, 0:bn], r1[:, 0:bn], AF.Relu, bias=c14[:, 0:1], scale=-1.0), [r1, c14], [r2])
                            S.op("dve", lambda: nc.vector.scalar_tensor_tensor(ng[:, 0:bn], r3[:, 0:bn], -7.0, sg_[:, 0:bn], ALU.add, ALU.mult), [r3, sg_], [ng])
                            S.op("dve", lambda: nc.vector.scalar_tensor_tensor(ng[:, 0:bn], r2[:, 0:bn], -6.0, ng[:, 0:bn], ALU.add, ALU.mult), [r2, ng], [ng])
                            S.op("dve", lambda: nc.vector.tensor_tensor(aT[:, j, 0:bn], ng[:, 0:bn], cb[:, b0:b0 + bn], ALU.mult), [ng, cb], [f"{aT.name}j{j}"])
                        for c in range(8):
                            p_ = po[c % 2]
                            if e == 0:
                                S.op("pe", lambda: nc.tensor.matmul(p_[:, 0:bn], b2b[:, c * 128:(c + 1) * 128], cTb[:, b0:b0 + bn], start=True, stop=False), [b2b, cTb], [p_])
                            for j in range(8):
                                S.op("pe", lambda: nc.tensor.matmul(p_[:, 0:bn], W2[:, j, c * 128:(c + 1) * 128], aT[:, j, 0:bn], start=(j == 0 and e != 0), stop=(j == 7)), [W2, f"{aT.name}j{j}"], [p_])
                            if getattr(self, "moe_pe_only", False): continue
                            if e == 0:
                                S.op("dve", lambda: nc.vector.tensor_copy(acc[:, c, b0:b0 + bn], p_[:, 0:bn]), [p_], [f"acc{c}"])
                            else:
                                S.op("dve", lambda: nc.vector.tensor_tensor(acc[:, c, b0:b0 + bn], acc[:, c, b0:b0 + bn], p_[:, 0:bn], ALU.add), [p_, f"acc{c}"], [f"acc{c}"])
                for ti in range(gsz):
                    t = t0 + ti
                    isc = t >= NTL
                    v = 1 if isc else 0
                    S.dma("sp", x1t[:], dr["x1"][t * 128:(t + 1) * 128, :], writes=[x1t])
                    for c in range(8):
                        S.op("pe", lambda: nc.tensor.transpose(pF[c // 4][:, c % 4, :], acc[:, c, ti * 128:(ti + 1) * 128], self.idf[:]), [f"acc{c}", self.idf], [pF[c // 4]])
                    for hh in range(2):
                        S.op("dve", lambda: nc.vector.tensor_tensor(tmpf[:, hh * 512:(hh + 1) * 512], pF[hh][:].rearrange("p a b -> p (a b)"), G6[v][:, hh * 512:(hh + 1) * 512], ALU.mult), [pF[hh], G6[v]], [f"tmpf{hh}"])
                        S.op("dve", lambda: nc.vector.tensor_tensor(x2t[:, hh * 512:(hh + 1) * 512], tmpf[:, hh * 512:(hh + 1) * 512], x1t[:, hh * 512:(hh + 1) * 512], ALU.add), [f"tmpf{hh}", x1t], [x2t])
                    if last:
                        S.dma("sp", self.out[t * 128:(t + 1) * 128, :], x2t[:], reads=[x2t], is_output=True)
                    else:
                        S.dma("sp", dr["xs"][t * 128:(t + 1) * 128, :], x2t[:], reads=[x2t])
                t0 += gsz
            S.barrier()

    def stage_dn(self, l, with_ctx):
        nc, S, dr = self.nc, self.S, self.dram
        NCH = T // 64
        with contextlib.ExitStack() as st:
            sb = lambda name, shape, dt=F32: st.enter_context(self.sbuf(name, shape, dt))
            ps = lambda name, shape, dt=F32: st.enter_context(self.psum(name, shape, dt))
            cw = sb("cw", [128, 6, 5]); blk = sb("blk64", [128, 128])
            S.dma("sp", cw[:], dr["dn_conv_l"][l], writes=[cw])
            S.dma("sp", blk[:], dr["blk64"][:, :], writes=[blk])
            xr = [sb(f"xr{i}", [128, T]) for i in range(2)]
            xc = [sb(f"xc{i}", [128, T]) for i in range(2)]
            sqt = [sb(f"sqt{i}", [128, 512]) for i in range(2)]
            rs = [sb(f"rs{i}", [128, 512]) for i in range(2)]
            pn = [ps(f"pn{i}", [128, 512]) for i in range(2)]
            bi = 0
            for c in range(6):
                r_, c_ = xr[c % 2], xc[c % 2]
                S.dma("sp", r_[:], dr["dnraw"][c * 128:(c + 1) * 128, :], writes=[r_])
                for (a, b) in ((0, NLAT), (NLAT, T)):
                    S.op("dve", lambda: nc.vector.tensor_scalar(c_[:, a:b], r_[:, a:b], cw[:, c, 2:3], None, ALU.mult), [r_, cw], [c_])
                    for s_ in (0, 1, 3, 4):
                        off = s_ - 2
                        lo, hi = a + max(0, -off), b - max(0, off)
                        S.op("dve", lambda: nc.vector.scalar_tensor_tensor(c_[:, lo:hi], r_[:, lo + off:hi + off], cw[:, c, s_:s_ + 1], c_[:, lo:hi], ALU.mult, ALU.add), [r_, cw, c_], [c_])
                S.op("act", lambda: nc.scalar.activation(c_[:], c_[:], AF.Silu), [c_], [c_])
                if c < 4:
                    for b0 in range(0, T, 512):
                        bn = min(512, T - b0)
                        q_, r2, p_ = sqt[bi % 2], rs[bi % 2], pn[bi % 2]; bi += 1
                        S.op("act", lambda: nc.scalar.activation(q_[:, 0:bn], c_[:, b0:b0 + bn], AF.Square), [c_], [q_])
                        S.op("pe", lambda: nc.tensor.matmul(p_[:, 0:bn], blk[:], q_[:, 0:bn], start=True, stop=True), [blk, q_], [p_])
                        S.op("act", lambda: nc.scalar.activation(r2[:, 0:bn], p_[:, 0:bn], AF.Sqrt, scale=1.0, bias=EPS), [p_], [r2])
                        S.op("dve", lambda: nc.vector.reciprocal(r2[:, 0:bn], r2[:, 0:bn]), [r2], [r2])
                        S.op("dve", lambda: nc.vector.scalar_tensor_tensor(c_[:, b0:b0 + bn], c_[:, b0:b0 + bn], (0.125 if c < 2 else 1.0), r2[:, 0:bn], ALU.mult, ALU.mult), [c_, r2], [c_])
                S.dma("sp", dr["dnT2"].rearrange("h d n -> (h d) n")[c * 128:(c + 1) * 128, :], c_[:], reads=[c_])
            S.barrier()
        with contextlib.ExitStack() as st:
            sb = lambda name, shape, dt=F32: st.enter_context(self.sbuf(name, shape, dt))
            ps = lambda name, shape, dt=F32: st.enter_context(self.psum(name, shape, dt))
            dc = sb("dc", [64, 7, 8, 64])
            S.dma("sp", dc[:], dr["dn_consts"][:, :, :, :], writes=[dc])
            C_tri, C_mT, C_mN, C_sT, C_sN, C_id = (dc[:, i] for i in range(6))
            ones64 = dc[:, 6, 0, :]
            ba = sb("ba", [64, NCH, 16])
            S.dma("sp", ba[:], dr["dn_ba"].rearrange("(n c) e -> c n e", c=64), writes=[ba])
            alog = sb("alog", [64, 8]); dtb = sb("dtb", [64, 8])
            S.dma("sp", alog[:], dr["dn_a_log"][l].partition_broadcast(64), writes=[alog])
            S.dma("sp", dtb[:], dr["dn_dt_bias"][l].partition_broadcast(64), writes=[dtb])
            Q = sb("Qs", [64, NCH, 8, 6])
            z = sb("z", [64, NCH, 8]); az = sb("az", [64, NCH, 8]); t3 = sb("t3", [64, NCH, 8])
            pb = [ps(f"pb{i}", [64, 8, 64]) for i in range(8)]
            pgA, pgB = pb[0][:].rearrange("p a c -> p (a c)"), pb[1][:].rearrange("p a c -> p (a c)")
            pgL = [pb[2][:].rearrange("p a c -> p (a c)"), pb[3][:].rearrange("p a c -> p (a c)")]
            bcn = lambda t_: t_[:].unsqueeze(1).to_broadcast([64, NCH, 8])
            S.op("act", lambda: nc.scalar.activation(alog[:], alog[:], AF.Exp), [alog], [alog])
            S.op("dve", lambda: nc.vector.tensor_scalar(alog[:], alog[:], -1.0, None, ALU.mult), [alog], [alog])
            S.op("act", lambda: nc.scalar.activation(Q[:, :, :, 0], ba[:, :, 0:8], AF.Sigmoid), [ba], ["Q0"])
            S.op("dve", lambda: nc.vector.tensor_tensor(z[:], ba[:, :, 8:16], bcn(dtb), ALU.add), [ba, dtb], [z])
            S.op("act", lambda: nc.scalar.activation(az[:], z[:], AF.Abs), [z], [az])
            S.op("act", lambda: nc.scalar.activation(az[:], az[:], AF.Exp, scale=-1.0), [az], [az])
            S.op("act", lambda: nc.scalar.activation(az[:], az[:], AF.Ln, bias=1.0, scale=1.0), [az], [az])
            S.op("dve", lambda: nc.vector.tensor_scalar(z[:], z[:], 0.0, None, ALU.max), [z], [z])
            S.op("dve", lambda: nc.vector.tensor_tensor(z[:], z[:], az[:], ALU.add), [z, az], [z])
            S.op("dve", lambda: nc.vector.tensor_tensor(Q[:, :, :, 5], z[:], bcn(alog), ALU.mult), [z, alog], ["Q5"])
            S.op("dve", lambda: nc.vector.tensor_copy(z[:], Q[:, :, :, 5]), ["Q5"], [z])
            S.op("pe", lambda: nc.tensor.matmul(pgA[:, 0:NCH * 4].rearrange("p (n h) -> p n h", h=4), dc[:, 0, 0, :], z[:, :, 0:4], start=True, stop=True), [dc, z], [pgA])
            S.op("pe", lambda: nc.tensor.matmul(pgB[:, 0:NCH * 4].rearrange("p (n h) -> p n h", h=4), dc[:, 0, 4, :], z[:, :, 4:8], start=True, stop=True), [dc, z], [pgB])
            S.op("dve", lambda: nc.vector.tensor_copy(Q[:, :, 0:4, 1], pgA[:, 0:NCH * 4].rearrange("p (n h) -> p n h", h=4)), [pgA], ["Q1"])
            S.op("dve", lambda: nc.vector.tensor_copy(Q[:, :, 4:8, 1], pgB[:, 0:NCH * 4].rearrange("p (n h) -> p n h", h=4)), [pgB], ["Q1"])
            hf_ = NCH // 2
            for i in range(2):
                S.op("pe", lambda: nc.tensor.matmul(pgL[i][:, 0:hf_ * 8].rearrange("p (n h) -> p n h", h=8), ones64, z[:, i * hf_:(i + 1) * hf_, :], start=True, stop=True), [dc, z], [pgL[i]])
                gl = pgL[i][:, 0:hf_ * 8].rearrange("p (n h) -> p n h", h=8)
                sl = slice(i * hf_, (i + 1) * hf_)
                S.op("dve", lambda: nc.vector.tensor_tensor(t3[:, sl, :], gl, Q[:, sl, :, 1], ALU.subtract), [pgL[i], "Q1"], [t3])
                S.op("act", lambda: nc.scalar.activation(Q[:, sl, :, 4], gl, AF.Exp), [pgL[i]], ["Q4"])
            S.op("act", lambda: nc.scalar.activation(Q[:, :, :, 3], t3[:], AF.Exp), [t3], ["Q3"])
            S.op("act", lambda: nc.scalar.activation(az[:], Q[:, :, :, 1], AF.Exp), ["Q1"], [az])
            S.op("dve", lambda: nc.vector.tensor_tensor(Q[:, :, :, 2], az[:], Q[:, :, :, 0], ALU.mult), [az, "Q0"], ["Q2"])
            QK = ["Q0", "Q1", "Q2", "Q3", "Q4", "Q5"]
            T8 = lambda name: sb(name, [64, 8, 64])
            Dq = [sb(f"Dq{i}", [64, 2, 12, 64]) for i in range(2)]
            SSt = sb("SSt", [64, 8, 6])
            X, Dn, EG, decT, decN, dgb, t1, t2 = (T8(n) for n in ("X", "Dn", "EG", "decT", "decN", "dgb", "t1", "t2"))
            T8b = lambda name: sb(name, [64, 8, 64], BF16)
            Bm = [T8b("Bm0"), T8b("Bm1")]; Am = [T8b("Am0"), T8b("Am1")]
            attnT, kdec, u, wT, qgT, vnew, St, osb = (T8(n) for n in ("attnT", "kdec", "u", "wT", "qgT", "vnew", "St", "osb"))
            Pm, vb, kbg = T8b("Pm"), T8b("vb"), T8b("kbg")
            S.op("pool", lambda: nc.gpsimd.memset(St[:], 0.0), [], [St])
            Fw = list(range(NLAT // 64, NCH)) + list(range(NLAT // 64))
            Bw = list(range(NCH - 1, NLAT // 64 - 1, -1)) + list(range(NLAT // 64 - 1, -1, -1))
            v4 = lambda ap: ap.rearrange("p (a b) c -> p a b c", a=2)
            f2 = lambda ap: ap.rearrange("p a c -> p (a c)")
            idf64 = self.idf[0:64, 0:64]
            dsrc = dr["dnT2"].rearrange("h d n -> d h n")
            for s_ in range(NCH):
                f, b = Fw[s_], Bw[s_]
                D_ = Dq[s_ % 2]
                S.dma("sp", D_[:, 0], dsrc[:, :, f * 64:(f + 1) * 64], writes=[D_])
                S.dma("act", D_[:, 1], dsrc[:, :, b * 64:(b + 1) * 64], writes=[D_])
                S.op("pool", lambda: nc.gpsimd.tensor_copy(SSt[:, 0:4, :], Q[:, f, 0:4, :]), QK, [SSt])
                S.op("pool", lambda: nc.gpsimd.tensor_copy(SSt[:, 4:8, :], Q[:, b, 4:8, :]), QK, [SSt])
                bc = lambda q: SSt[:, :, q].unsqueeze(2).to_broadcast([64, 8, 64])
                qTs, kTs, vTs = D_[:, :, 0:4, :], D_[:, :, 4:8, :], D_[:, :, 8:12, :]
                slot = lambda base, i: D_[:, i // 4, base + i % 4, :]
                pA, pB, pK, pV, pKK, pKQ, pP_, pX = pb
                S.op("dve", lambda: nc.vector.tensor_tensor(X[:], C_tri, bc(5), ALU.mult), [dc, SSt], [X])
                S.op("pe", lambda: nc.tensor.matmul(f2(pA[:]), ones64, f2(X[:]), start=True, stop=True), [dc, X], [pA])
                S.op("dve", lambda: nc.vector.tensor_tensor(Dn[:], pA[:], bc(1), ALU.subtract), [pA, SSt], [Dn])
                S.op("act", lambda: nc.scalar.activation(EG[:], pA[:], AF.Exp), [pA], [EG])
                S.op("dve", lambda: nc.vector.tensor_tensor(decT[:], Dn[:], C_mT, ALU.add), [Dn, dc], [decT])
                S.op("act", lambda: nc.scalar.activation(decT[:], decT[:], AF.Exp), [decT], [decT])
                S.op("dve", lambda: nc.vector.scalar_tensor_tensor(decN[:], Dn[:], -1.0, C_mN, ALU.mult, ALU.add), [Dn, dc], [decN])
                S.op("act", lambda: nc.scalar.activation(decN[:], decN[:], AF.Exp), [decN], [decN])
                S.op("pool", lambda: nc.gpsimd.tensor_tensor(dgb[:], C_id, bc(0), ALU.mult), [dc, SSt], [dgb])
                S.op("pe", lambda: nc.tensor.matmul(f2(pB[:]), ones64, f2(dgb[:]), start=True, stop=True), [dc, dgb], [pB])
                for i in range(8):
                    S.op("pe", lambda: nc.tensor.transpose(pK[:, i, :], slot(4, i), idf64), [D_, self.idf], [pK])
                for i in range(8):
                    S.op("pe", lambda: nc.tensor.transpose(pV[:, i, :], slot(8, i), idf64), [D_, self.idf], [pV])
                for i in range(8):
                    S.op("pe", lambda: nc.tensor.matmul(pKK[:, i, :], slot(4, i), slot(4, i), start=True, stop=True), [D_], [pKK])
                for i in range(8):
                    S.op("pe", lambda: nc.tensor.matmul(pKQ[:, i, :], slot(4, i), slot(0, i), start=True, stop=True), [D_], [pKQ])
                S.op("pool", lambda: nc.gpsimd.tensor_tensor(t1[:], decT[:], C_sT, ALU.mult), [decT, dc], [t1])
                S.op("dve", lambda: nc.vector.tensor_tensor(t1[:], t1[:], pB[:], ALU.mult), [t1, pB], [t1])
                S.op("dve", lambda: nc.vector.tensor_tensor(Bm[0][:], t1[:], pKK[:], ALU.mult), [t1, pKK], [Bm[0]])
                S.op("pool", lambda: nc.gpsimd.tensor_tensor(t2[:], decN[:], C_sN, ALU.mult), [decN, dc], [t2])
                S.op("pool", lambda: nc.gpsimd.tensor_tensor(t2[:], t2[:], bc(0), ALU.mult), [t2, SSt], [t2])
                S.op("dve", lambda: nc.vector.tensor_tensor(Am[0][:], t2[:], pKK[:], ALU.mult), [t2, pKK], [Am[0]])
                S.op("dve", lambda: nc.vector.tensor_tensor(attnT[:], pKQ[:], decT[:], ALU.mult), [pKQ, decT], [attnT])
                S.op("pool", lambda: nc.gpsimd.tensor_tensor(Pm[:], C_id, Bm[0][:], ALU.subtract), [dc, Bm[0]], [Pm])
                cur = 0
                for r in range(1, 6):
                    nx = 1 - cur
                    if r < 5:
                        for i in range(8):
                            S.op("pe", lambda: nc.tensor.matmul(pA[:, i, :], Am[cur][:, i, :], Bm[cur][:, i, :], start=True, stop=True), [Am[cur], Bm[cur]], [pA])
                    for i in range(8):
                        S.op("pe", lambda: nc.tensor.matmul(pB[:, i, :], Bm[cur][:, i, :], Am[cur][:, i, :], start=True, stop=True), [Am[cur], Bm[cur]], [pB])
                    if r < 5:
                        S.op("act", lambda: nc.scalar.copy(Bm[nx][:], pA[:]), [pA], [Bm[nx]])
                    S.op("dve", lambda: nc.vector.tensor_copy(Am[nx][:], pB[:]), [pB], [Am[nx]])
                    for i in range(8):
                        S.op("pe", lambda: nc.tensor.matmul(pP_[:, i, :], Am[nx][:, i, :], Pm[:, i, :], start=True, stop=True), [Am[nx], Pm], [pP_])
                    S.op("dve", lambda: nc.vector.tensor_tensor(Pm[:], Pm[:], pP_[:], ALU.add), [Pm, pP_], [Pm])
                    cur = nx
                S.op("dve", lambda: nc.vector.tensor_tensor(vb[:], pV[:], bc(0), ALU.mult), [pV, SSt], [vb])
                S.op("dve", lambda: nc.vector.tensor_tensor(kbg[:], pK[:], bc(2), ALU.mult), [pK, SSt], [kbg])
                S.op("dve", lambda: nc.vector.tensor_tensor(kdec[:], pK[:], bc(3), ALU.mult), [pK, SSt], [kdec])
                for i in range(8):
                    S.op("pe", lambda: nc.tensor.matmul(pKK[:, i, :], Pm[:, i, :], vb[:, i, :], start=True, stop=True), [Pm, vb], [pKK])
                S.op("act", lambda: nc.scalar.copy(u[:], pKK[:]), [pKK], [u])
                for i in range(8):
                    S.op("pe", lambda: nc.tensor.matmul(pKQ[:, i, :], kbg[:, i, :], Pm[:, i, :], start=True, stop=True), [Pm, kbg], [pKQ])
                S.op("act", lambda: nc.scalar.copy(wT[:], pKQ[:]), [pKQ], [wT])
                S.op("pool", lambda: nc.gpsimd.tensor_tensor(v4(qgT[:]), qTs, v4(EG[:]), ALU.mult), [D_, EG], [qgT])
                for i in range(8):
                    S.op("pe", lambda: nc.tensor.matmul(pKK[:, i, :], wT[:, i, :], St[:, i, :], start=True, stop=True), [wT, St], [pKK])
                S.op("dve", lambda: nc.vector.tensor_tensor(vnew[:], u[:], pKK[:], ALU.subtract), [u, pKK], [vnew])
                for i in range(8):
                    S.op("pe", lambda: nc.tensor.matmul(pKQ[:, i, :], qgT[:, i, :], St[:, i, :], start=True, stop=False), [qgT, St], [pKQ])
                    S.op("pe", lambda: nc.tensor.matmul(pKQ[:, i, :], attnT[:, i, :], vnew[:, i, :], start=False, stop=True), [attnT, vnew], [pKQ])
                for i in range(8):
                    S.op("pe", lambda: nc.tensor.matmul(pP_[:, i, :], kdec[:, i, :], vnew[:, i, :], start=True, stop=True), [kdec, vnew], [pP_])
                S.op("pool", lambda: nc.gpsimd.tensor_tensor(St[:], St[:], bc(4), ALU.mult), [St, SSt], [St])
                S.op("dve", lambda: nc.vector.tensor_tensor(St[:], St[:], pP_[:], ALU.add), [St, pP_], [St])
                S.op("act", lambda: nc.scalar.copy(osb[:], pKQ[:]), [pKQ], [osb])
                S.dma("sp", dr["o_dn"][0, f * 64:(f + 1) * 64, :].rearrange("p (h e) -> p h e", h=4), osb[:, 0:4, :], reads=[osb])
                S.dma("sp", dr["o_dn"][1, b * 64:(b + 1) * 64, :].rearrange("p (h e) -> p h e", h=4), osb[:, 4:8, :], reads=[osb])
            S.barrier()
        with contextlib.ExitStack() as st:
            sb = lambda name, shape, dt=F32: st.enter_context(self.sbuf(name, shape, dt))
            ps = lambda name, shape, dt=F32: st.enter_context(self.psum(name, shape, dt))
            gd = sb("gdn", [128, 4, 64])
            gt = self.load_bcast(st, "gt_dn", dr["dn_out_g"][l], 64)
            S.op("dve", lambda: nc.vector.tensor_copy(gd[:], gt[:].unsqueeze(1).to_broadcast([128, 4, 64])), [gt], [gd])
            of = [sb(f"of{i}", [128, 256]) for i in range(2)]; ob = [sb(f"ob{i}", [128, 256]) for i in range(2)]
            gte = [sb(f"gte{i}", [128, 256]) for i in range(2)]
            sq = sb("sqd", [128, 256]); ss = sb("ssd", [128, 4]); tmpn = sb("tmpd", [128, 256]); yn = sb("ynd", [128, 256])
            ybf = sb("ybf", [128, 256], BF16); ySb = sb("ySb", [128, 2, 128], BF16)
            pT = ps("pTd", [128, 8, 128], BF16)
            v3 = lambda ap: ap.rearrange("p (h d) -> p h d", h=4)
            for t in range(NT if with_ctx else NTL):
                a_, b_, g_ = of[t % 2], ob[t % 2], gte[t % 2]
                S.dma("sp", a_[:], dr["o_dn"][0, t * 128:(t + 1) * 128, :], writes=[a_])
                S.dma("act", b_[:], dr["o_dn"][1, t * 128:(t + 1) * 128, :], writes=[b_])
                S.dma("sp", g_[:], dr["dn_gate"][t * 128:(t + 1) * 128, :], writes=[g_])
                S.op("pool", lambda: nc.gpsimd.tensor_tensor(a_[:], a_[:], b_[:], ALU.add), [a_, b_], [a_])
                self.headnorm_k(v3(a_[:]), [a_], 4, 64, gd[:], v3(yn[:]), v3(sq[:]), ss[:], v3(tmpn[:]))
                S.op("act", lambda: nc.scalar.activation(g_[:], g_[:], AF.Silu), [g_], [g_])
                S.op("dve", lambda: nc.vector.tensor_tensor(ybf[:], yn[:], g_[:], ALU.mult), [yn, g_], [ybf])
                for k in range(2):
                    S.op("pe", lambda: nc.tensor.transpose(pT[:, k, :], ybf[:, k * 128:(k + 1) * 128], self.idb[:]), [ybf, self.idb], [pT])
                S.op("act", lambda: nc.scalar.copy(ySb[:], pT[:, 0:2, :]), [pT], [ySb])
                S.dma("sp", dr["yT"][512:768, :].rearrange("(k p) n -> p k n", p=128)[:, :, t * 128:(t + 1) * 128], ySb[:], reads=[ySb])
            S.barrier()


def _dn_consts():
    t = np.arange(64)
    c = np.zeros((64, 7, 8, 64), np.float32)
    le = (t[:, None] <= t[None, :]).astype(np.float32)
    ge = (t[:, None] >= t[None, :]).astype(np.float32)
    lt = (t[:, None] < t[None, :]).astype(np.float32)
    gt = (t[:, None] > t[None, :]).astype(np.float32)
    eye = np.eye(64, dtype=np.float32)
    for s in range(8):
        fwd = s < 4
        c[:, 0, s, :] = le if fwd else ge
        c[:, 1, s, :] = (1 - (le if fwd else ge)) * MASKNEG
        c[:, 2, s, :] = (1 - (ge if fwd else le)) * MASKNEG
        c[:, 3, s, :] = lt if fwd else gt
        c[:, 4, s, :] = gt if fwd else lt
        c[:, 5, s, :] = eye
        c[:, 6, s, :] = 1.0
    return c


def _shared_inputs(inp):
    f = lambda a: np.ascontiguousarray(np.asarray(a, dtype=np.float32))
    sh = {}
    for k in ("ada_w", "ada_b", "norm1_g", "norm2_g", "w_in", "w_out", "na_qn_g", "na_kn_g", "gqa_qn_g", "gqa_kn_g",
              "dn_out_g", "mla_cq_g", "mla_ckv_g", "mla_w_uq", "mla_w_ukv", "mla_qn_g", "mla_kn_g", "router_w",
              "router_b", "exp_w1", "exp_w2", "exp_b2"):
        sh[k] = f(inp[k])
    rb = f(inp["na_rel_bias"])
    g = rb[:, :, _NA_DR, _NA_DC]
    g = np.where(_NA_OK[None, None], g, np.float32(MASKNEG)).astype(np.float32)
    sh["na_bias"] = np.ascontiguousarray(g.transpose(0, 2, 1, 3, 4))
    cw = f(inp["dn_conv_w"])
    sh["dn_conv_l"] = np.ascontiguousarray(cw.reshape(DEPTH, 5, 6, 128).transpose(0, 3, 2, 1))
    sh["dn_a_log"] = f(inp["dn_a_log"]).reshape(DEPTH, 8)
    sh["dn_dt_bias"] = f(inp["dn_dt_bias"]).reshape(DEPTH, 8)
    b1 = f(inp["exp_b1"])
    sh["exp_b1_l"] = np.ascontiguousarray(b1.reshape(DEPTH, NE, 16, 128).transpose(0, 3, 1, 2))
    cg, sg = _rope_tables(64)
    cm, sm = _rope_tables(32)
    sh["cos_g"], sh["sin_g"], sh["cos_m"], sh["sin_m"] = cg, sg, cm, sm
    sh["dn_consts"] = _dn_consts()
    s65 = np.zeros((65, 64), np.float32); s65[64, :] = 1.0
    sh["sel65"] = s65
    bk = np.zeros((128, 128), np.float32); bk[:64, :64] = 1.0; bk[64:, 64:] = 1.0
    sh["blk64"] = bk
    sh["ident"] = np.eye(128, dtype=np.float32)
    sh["cctx_l"] = np.ascontiguousarray(f(inp["c_ctx"]).reshape(8, 128).T)
    return sh


def _core_inputs(inp, sh, b):
    m = dict(sh)
    m["x"] = np.ascontiguousarray(np.asarray(inp["x"][b], dtype=np.float32))
    m["ctx"] = np.ascontiguousarray(np.asarray(inp["ctx"][b], dtype=np.float32))
    m["c_l"] = np.ascontiguousarray(np.asarray(inp["c"][b], dtype=np.float32).reshape(8, 128).T)
    return m


_NC_CACHE = {}


def kernel(**inputs):
    if "nc" not in _NC_CACHE:
        _NC_CACHE["nc"] = Builder().build()
    nc = _NC_CACHE["nc"]
    sh = _shared_inputs(inputs)
    B = inputs["x"].shape[0]
    in_maps = [_core_inputs(inputs, sh, b) for b in range(B)]
    res = run_bass_kernel_spmd(nc, in_maps, core_ids=list(range(B)))
    return np.stack([np.asarray(r["out"], dtype=np.float32) for r in res.results], axis=0)
```
